# Optimizing a Trainium2 kernel written in Bass

```python
import math
import jax, jax.numpy as jnp
from jax import lax
import numpy as np

D_MODEL = 2048
BATCH = 4
SEQ = 4096
DEPTH = 2

GRID_W = 64
CTX_LEN = 256
Q_BLOCK = 128
ROPE_BASE = 10000.0
EPS = 1e-6

MLA_HEADS = 8
MLA_Q_LORA = 512
MLA_KV_LORA = 512
MLA_NOPE = 128
MLA_ROPE = 64
MLA_V = 128

DIFF_HEADS = 4
DIFF_DIM = 128
DIFF_QK = 2 * DIFF_HEADS * DIFF_DIM

EVEN_SPLITS = (MLA_Q_LORA,
               MLA_Q_LORA + MLA_KV_LORA,
               MLA_Q_LORA + MLA_KV_LORA + MLA_ROPE,
               MLA_Q_LORA + MLA_KV_LORA + MLA_ROPE + DIFF_QK,
               MLA_Q_LORA + MLA_KV_LORA + MLA_ROPE + 2 * DIFF_QK)
EVEN_IN = MLA_Q_LORA + MLA_KV_LORA + MLA_ROPE + 3 * DIFF_QK
EVEN_OUT = MLA_HEADS * MLA_V + DIFF_HEADS * 2 * DIFF_DIM

POOL_WINDOWS = (2, 4, 8, 16)
POOL_WIDTH = D_MODEL // 2
POOL_GROUP = POOL_WIDTH // 4
FOURIER_GROUPS = 4
FOURIER_WIDTH = D_MODEL - POOL_WIDTH
FOURIER_GROUP = FOURIER_WIDTH // FOURIER_GROUPS
ODD_WIDTH = POOL_WIDTH + FOURIER_WIDTH

N_EXPERTS = 16
EC_CAPACITY = 2
EXPERT_FF = D_MODEL

kernel_name = 'hybrid_mla_diffattn_pool_fnet_ecmoe_dit'


def rms_norm(x, g):
    xf = x.astype(jnp.float32)
    y = xf * lax.rsqrt(jnp.mean(xf * xf, axis=-1, keepdims=True) + EPS)
    return (y * g.astype(jnp.float32)).astype(x.dtype)


def rope_2d_tables(n, rot_dim):
    rows = n // GRID_W
    row = jnp.repeat(jnp.arange(rows, dtype=jnp.float32), GRID_W)
    col = jnp.tile(jnp.arange(GRID_W, dtype=jnp.float32), rows)
    quarter = rot_dim // 4
    inv_freq = ROPE_BASE ** (-jnp.arange(quarter, dtype=jnp.float32) / quarter)
    ang = jnp.stack([row[:, None] * inv_freq, col[:, None] * inv_freq], axis=1)
    return jnp.cos(ang)[:, None], jnp.sin(ang)[:, None]


def apply_rope_2d(x, cos, sin):
    shape = x.shape
    r = shape[-1]
    xr = x.reshape(shape[0], shape[1], -1, 2, 2, r // 4).astype(jnp.float32)
    x1, x2 = xr[..., 0, :], xr[..., 1, :]
    out = jnp.stack([x1 * cos - x2 * sin, x2 * cos + x1 * sin], axis=-2)
    return out.reshape(shape).astype(x.dtype)


def block_attention(q, k, v, scale):
    b, nq, m, h, dk = q.shape
    nb = nq // Q_BLOCK
    qb = jnp.moveaxis(q.reshape(b, nb, Q_BLOCK, m, h, dk), 1, 0)

    def one_block(qi):
        s = jnp.einsum('bqmhd,bkmhd->bmhqk', qi, k, preferred_element_type=jnp.float32) * scale
        p = jax.nn.softmax(s, axis=-1).astype(v.dtype)
        return jnp.einsum('bmhqk,bkhd->bqmhd', p, v)

    o = lax.map(one_block, qb)
    return jnp.moveaxis(o, 0, 1).reshape(b, nq, m, h, v.shape[-1])


def even_project(h, w_in, q_norm, w_uq, kv_norm, w_ukv, rope_a, rope_b, need_q):
    b, n, _ = h.shape
    cq, ckv, kr, qd, kd, vd = jnp.split(h @ w_in, EVEN_SPLITS, axis=-1)
    kv = (rms_norm(ckv, kv_norm) @ w_ukv).reshape(b, n, MLA_HEADS, MLA_NOPE + MLA_V)
    k_nope, v_a = kv[..., :MLA_NOPE], kv[..., MLA_NOPE:]
    k_rope = kr.reshape(b, n, 1, MLA_ROPE)
    kd = kd.reshape(b, n, 2, DIFF_HEADS, DIFF_DIM)
    vd = vd.reshape(b, n, DIFF_HEADS, 2 * DIFF_DIM)
    if rope_a is not None:
        k_rope = apply_rope_2d(k_rope, *rope_a)
        kd = apply_rope_2d(kd, *rope_b)
    k_a = jnp.concatenate([k_nope, jnp.broadcast_to(k_rope, (b, n, MLA_HEADS, MLA_ROPE))], axis=-1)[:, :, None]
    if not need_q:
        return None, k_a, v_a, None, kd, vd
    q = (rms_norm(cq, q_norm) @ w_uq).reshape(b, n, MLA_HEADS, MLA_NOPE + MLA_ROPE)
    q_nope, q_rope = q[..., :MLA_NOPE], q[..., MLA_NOPE:]
    qd = qd.reshape(b, n, 2, DIFF_HEADS, DIFF_DIM)
    if rope_a is not None:
        q_rope = apply_rope_2d(q_rope, *rope_a)
        qd = apply_rope_2d(qd, *rope_b)
    q_a = jnp.concatenate([q_nope, q_rope], axis=-1)[:, :, None]
    return q_a, k_a, v_a, qd, kd, vd


def even_heads_out(o_a, o_d, lam, lam_init, subln, w_out):
    b, n = o_a.shape[:2]
    od = o_d[:, :, 0] - lam.astype(o_d.dtype) * o_d[:, :, 1]
    od = rms_norm(od, subln) * (1.0 - lam_init)
    y = jnp.concatenate([o_a.reshape(b, n, -1), od.reshape(b, n, -1)], axis=-1)
    return y @ w_out


def even_mixer(h_lat, h_ctx, layer_idx, w_in, q_norm, w_uq, kv_norm, w_ukv,
               lq1, lk1, lq2, lk2, subln, w_out, with_ctx_out):
    n = h_lat.shape[1]
    rope_a = rope_2d_tables(n, MLA_ROPE)
    rope_b = rope_2d_tables(n, DIFF_DIM)
    qa_l, ka_l, va_l, qd_l, kd_l, vd_l = even_project(h_lat, w_in, q_norm, w_uq, kv_norm, w_ukv, rope_a, rope_b, True)
    qa_c, ka_c, va_c, qd_c, kd_c, vd_c = even_project(h_ctx, w_in, q_norm, w_uq, kv_norm, w_ukv, None, None, with_ctx_out)
    lam_init = 0.8 - 0.6 * math.exp(-0.3 * layer_idx)
    lam = (jnp.exp(jnp.sum(lq1.astype(jnp.float32) * lk1.astype(jnp.float32)))
           - jnp.exp(jnp.sum(lq2.astype(jnp.float32) * lk2.astype(jnp.float32))) + lam_init)
    scale_a = 1.0 / math.sqrt(MLA_NOPE + MLA_ROPE)
    scale_d = 1.0 / math.sqrt(DIFF_DIM)
    oa = block_attention(qa_l, jnp.concatenate([ka_c, ka_l], axis=1), jnp.concatenate([va_c, va_l], axis=1), scale_a)[:, :, 0]
    od = block_attention(qd_l, jnp.concatenate([kd_c, kd_l], axis=1), jnp.concatenate([vd_c, vd_l], axis=1), scale_d)
    y_lat = even_heads_out(oa, od, lam, lam_init, subln, w_out)
    y_ctx = None
    if with_ctx_out:
        oa_c = block_attention(qa_c, ka_c, va_c, scale_a)[:, :, 0]
        od_c = block_attention(qd_c, kd_c, vd_c, scale_d)
        y_ctx = even_heads_out(oa_c, od_c, lam, lam_init, subln, w_out)
    return y_lat, y_ctx


def multi_scale_pool(z):
    b, n, g, cg = z.shape
    zf = z.astype(jnp.float32)
    csum = jnp.concatenate([jnp.zeros((b, 1, g, cg), jnp.float32), jnp.cumsum(zf, axis=1)], axis=1)
    t = jnp.arange(n)[:, None]
    half = jnp.array(POOL_WINDOWS, dtype=jnp.int32)[None, :] // 2
    lo = jnp.clip(t - half, 0, n)
    hi = jnp.clip(t + half, 0, n)
    gidx = jnp.arange(g)[None, :]
    window_sum = csum[:, hi, gidx] - csum[:, lo, gidx]
    mean = window_sum / (hi - lo).astype(jnp.float32)[None, :, :, None]
    return (mean - zf).astype(z.dtype)


def odd_mixer(h, w_in, pool_w, pool_scale, fourier_w, w_out):
    b, n, _ = h.shape
    z = h @ w_in
    zp = z[..., :POOL_WIDTH].reshape(b, n, len(POOL_WINDOWS), POOL_GROUP)
    zf = z[..., POOL_WIDTH:].reshape(b, n, FOURIER_GROUPS, FOURIER_GROUP)
    yp = jnp.einsum('bngc,gcd->bngd', multi_scale_pool(zp), pool_w).reshape(b, n, POOL_WIDTH) * pool_scale
    spec = jnp.fft.fft2(zf.astype(jnp.float32), axes=(1, 3), norm='ortho').real.astype(z.dtype)
    yf = jnp.einsum('bngc,gcd->bngd', spec, fourier_w).reshape(b, n, FOURIER_WIDTH)
    return jnp.concatenate([yp, yf], axis=-1) @ w_out


def expert_choice_ffn(h, w_router, w_gate, w_up, w_down):
    b, n, d = h.shape
    cap = (EC_CAPACITY * n) // N_EXPERTS
    logits = jnp.einsum('bnd,de->ben', h, w_router, preferred_element_type=jnp.float32)
    aff = jax.nn.softmax(logits, axis=1)
    gate, idx = lax.top_k(aff, cap)
    xe = jax.vmap(lambda hb, ib: hb[ib])(h, idx)
    hid = jax.nn.silu(jnp.einsum('becd,edf->becf', xe, w_gate)) * jnp.einsum('becd,edf->becf', xe, w_up)
    ye = jnp.einsum('becf,efd->becd', hid, w_down) * gate[..., None].astype(h.dtype)
    return jax.vmap(lambda yb, ib: jnp.zeros((n, d), yb.dtype).at[ib.reshape(-1)].add(yb.reshape(-1, d)))(ye, idx)


def setup_inputs(seed: int = 0) -> dict:
    key = jax.random.key(seed)
    ks = iter(jax.random.split(key, 40))
    n_even = (DEPTH + 1) // 2
    n_odd = DEPTH // 2

    def nrm(shape, scale):
        return jax.random.normal(next(ks), shape, jnp.float32) * scale

    def gain(shape):
        return 1.0 + nrm(shape, 0.02)

    return {
        'x': nrm((BATCH, SEQ, D_MODEL), 1.0),
        'c': nrm((BATCH, D_MODEL), 1.0),
        'ctx': nrm((BATCH, CTX_LEN, D_MODEL), 1.0),
        'c_ctx': nrm((D_MODEL,), 1.0),
        'mod_w': nrm((DEPTH, D_MODEL, 6 * D_MODEL), 0.5 * D_MODEL ** -0.5),
        'mod_b': nrm((DEPTH, 6 * D_MODEL), 0.02),
        'norm_mix': gain((DEPTH, D_MODEL)),
        'norm_ffn': gain((DEPTH, D_MODEL)),
        'even_w_in': nrm((n_even, D_MODEL, EVEN_IN), D_MODEL ** -0.5),
        'mla_q_norm': gain((n_even, MLA_Q_LORA)),
        'mla_w_uq': nrm((n_even, MLA_Q_LORA, MLA_HEADS * (MLA_NOPE + MLA_ROPE)), MLA_Q_LORA ** -0.5),
        'mla_kv_norm': gain((n_even, MLA_KV_LORA)),
        'mla_w_ukv': nrm((n_even, MLA_KV_LORA, MLA_HEADS * (MLA_NOPE + MLA_V)), MLA_KV_LORA ** -0.5),
        'diff_lambda_q1': nrm((n_even, DIFF_DIM), 0.1),
        'diff_lambda_k1': nrm((n_even, DIFF_DIM), 0.1),
        'diff_lambda_q2': nrm((n_even, DIFF_DIM), 0.1),
        'diff_lambda_k2': nrm((n_even, DIFF_DIM), 0.1),
        'diff_subln': gain((n_even, 2 * DIFF_DIM)),
        'even_w_out': nrm((n_even, EVEN_OUT, D_MODEL), EVEN_OUT ** -0.5),
        'odd_w_in': nrm((n_odd, D_MODEL, ODD_WIDTH), D_MODEL ** -0.5),
        'pool_w': nrm((n_odd, len(POOL_WINDOWS), POOL_GROUP, POOL_GROUP), POOL_GROUP ** -0.5),
        'pool_scale': gain((n_odd, POOL_WIDTH)),
        'fourier_w': nrm((n_odd, FOURIER_GROUPS, FOURIER_GROUP, FOURIER_GROUP), FOURIER_GROUP ** -0.5),
        'odd_w_out': nrm((n_odd, ODD_WIDTH, D_MODEL), ODD_WIDTH ** -0.5),
        'router_w': nrm((DEPTH, D_MODEL, N_EXPERTS), D_MODEL ** -0.5),
        'expert_w_gate': nrm((DEPTH, N_EXPERTS, D_MODEL, EXPERT_FF), D_MODEL ** -0.5),
        'expert_w_up': nrm((DEPTH, N_EXPERTS, D_MODEL, EXPERT_FF), D_MODEL ** -0.5),
        'expert_w_down': nrm((DEPTH, N_EXPERTS, EXPERT_FF, D_MODEL), EXPERT_FF ** -0.5),
        'final_norm': gain((D_MODEL,)),
    }


def reference(x, c, ctx, c_ctx, mod_w, mod_b, norm_mix, norm_ffn,
              even_w_in, mla_q_norm, mla_w_uq, mla_kv_norm, mla_w_ukv,
              diff_lambda_q1, diff_lambda_k1, diff_lambda_q2, diff_lambda_k2, diff_subln, even_w_out,
              odd_w_in, pool_w, pool_scale, fourier_w, odd_w_out,
              router_w, expert_w_gate, expert_w_up, expert_w_down, final_norm):
    xl, xc = x, ctx
    for i in range(DEPTH):
        j = i // 2
        ctx_live = any(k % 2 == 0 for k in range(i + 1, DEPTH))
        ctx_in = (i % 2 == 0) or ctx_live
        ml = (jax.nn.silu(c) @ mod_w[i] + mod_b[i])[:, None, :]
        sh1, sc1, g1, sh2, sc2, g2 = jnp.split(ml, 6, axis=-1)
        hl = rms_norm(xl, norm_mix[i]) * (1.0 + sc1) + sh1
        if ctx_in:
            mc = jax.nn.silu(c_ctx) @ mod_w[i] + mod_b[i]
            csh1, csc1, cg1, csh2, csc2, cg2 = jnp.split(mc, 6, axis=-1)
            hc = rms_norm(xc, norm_mix[i]) * (1.0 + csc1) + csh1
        if i % 2 == 0:
            yl, yc = even_mixer(hl, hc, i, even_w_in[j], mla_q_norm[j], mla_w_uq[j], mla_kv_norm[j], mla_w_ukv[j],
                                diff_lambda_q1[j], diff_lambda_k1[j], diff_lambda_q2[j], diff_lambda_k2[j],
                                diff_subln[j], even_w_out[j], ctx_live)
        else:
            yl = odd_mixer(hl, odd_w_in[j], pool_w[j], pool_scale[j], fourier_w[j], odd_w_out[j])
            yc = odd_mixer(hc, odd_w_in[j], pool_w[j], pool_scale[j], fourier_w[j], odd_w_out[j]) if ctx_live else None
        xl = xl + g1 * yl
        hl = rms_norm(xl, norm_ffn[i]) * (1.0 + sc2) + sh2
        xl = xl + g2 * expert_choice_ffn(hl, router_w[i], expert_w_gate[i], expert_w_up[i], expert_w_down[i])
        if ctx_live:
            xc = xc + cg1 * yc
            hc = rms_norm(xc, norm_ffn[i]) * (1.0 + csc2) + csh2
            xc = xc + cg2 * expert_choice_ffn(hc, router_w[i], expert_w_gate[i], expert_w_up[i], expert_w_down[i])
    return rms_norm(xl, final_norm)
```

```python
import math
from contextlib import ExitStack
import numpy as np
import ml_dtypes
import concourse.bass as bass
import concourse.mybir as mybir
from concourse.bass_utils import run_bass_kernel_spmd

F32 = mybir.dt.float32
BF16 = mybir.dt.bfloat16
I32 = mybir.dt.int32
U32 = mybir.dt.uint32
AF = mybir.ActivationFunctionType
ALU = mybir.AluOpType
AX = mybir.AxisListType
NPBF = ml_dtypes.bfloat16

D = 2048
SEQ = 4096
NB = 4
CTX = 256
EPS = 1e-6
NCORES = 8
TOWN = 2048


class Buf:
    __slots__ = ("name", "w", "r", "sem", "issued")

    def __init__(self, name):
        self.name = name
        self.w = None
        self.r = {}
        self.sem = None
        self.issued = 0


ENG = {"pe": "tensor", "dve": "vector", "act": "scalar", "pool": "gpsimd", "sp": "sync"}


class Prog:
    def __init__(self, nc, stack):
        self.nc = nc
        self.stack = stack
        self.esem = {e: stack.enter_context(nc.semaphore("es_" + e)) for e in ENG}
        self.cnt = {e: 0 for e in ENG}
        self.seen = {e: {} for e in ENG}
        self.dbufs = []
        self.nsem = 5

    def eng(self, e):
        return getattr(self.nc, ENG[e])

    def _wait(self, e, key, sem, val):
        if self.seen[e].get(key, 0) >= val:
            return
        self.seen[e][key] = val
        self.eng(e).wait_ge(sem, val)

    def _deps(self, e, reads, writes):
        evs = {}

        def add(key, sem, val):
            if key not in evs or evs[key][1] < val:
                evs[key] = (sem, val)

        for b in reads:
            if b.w is not None:
                add(*b.w)
        for b in writes:
            if b.w is not None:
                add(*b.w)
            for k, (sem, val) in b.r.items():
                add(k, sem, val)
        for key, (sem, val) in evs.items():
            if isinstance(key, Buf):
                val = key.issued
            elif e == "pe" and key == "pe":
                continue
            self._wait(e, key, sem, val)

    def _post(self, ev, reads, writes):
        key, sem, val = ev
        for b in writes:
            b.w = ev
            b.r = {}
        for b in reads:
            if b in writes:
                continue
            b.r[key] = (sem, val)

    def op(self, e, fn, reads=(), writes=(), sig=True):
        self._deps(e, reads, writes)
        inst = fn(self.eng(e))
        if sig:
            self.cnt[e] += 1
            inst.then_inc(self.esem[e], 1)
            val = self.cnt[e]
        else:
            val = self.cnt[e] + 1
        self._post((e, self.esem[e], val), reads, writes)
        return inst

    def _dsem(self, sb):
        if sb.sem is None:
            sb.sem = self.stack.enter_context(self.nc.semaphore("ds%d" % len(self.dbufs)))
            self.dbufs.append(sb)
            self.nsem += 1
        return sb.sem

    def dma(self, q, out, in_, sb, reads=(), writes=(), **kw):
        self._deps(q, reads, writes)
        sem = self._dsem(sb)
        inst = self.eng(q).dma_start(out=out, in_=in_, **kw)
        sb.issued += 16
        inst.then_inc(sem, 16)
        self._post((sb, sem, sb.issued), reads, writes)
        return inst

    def idma(self, out, out_off, in_, in_off, sb, reads=(), writes=(), **kw):
        self._deps("pool", reads, writes)
        sem = self._dsem(sb)
        inst = self.nc.gpsimd.indirect_dma_start(out=out, out_offset=out_off, in_=in_, in_offset=in_off, **kw)
        sb.issued += 16
        inst.then_inc(sem, 16)
        self._post((sb, sem, sb.issued), reads, writes)
        return inst

    def barrier(self):
        for e in ENG:
            for e2 in ENG:
                if e2 != e and self.cnt[e2] > 0:
                    self._wait(e, e2, self.esem[e2], self.cnt[e2])
            for b in self.dbufs:
                if b.issued:
                    self._wait(e, b, b.sem, b.issued)

    def finish(self):
        for b in self.dbufs:
            if b.issued:
                self._wait("sp", b, b.sem, b.issued)
        for e in ENG:
            if e != "sp" and self.cnt[e] > 0:
                self._wait("sp", e, self.esem[e], self.cnt[e])


class Ctx:
    def __init__(self):
        self.nc = bass.Bass("TRN2", target_bir_lowering=False)
        self.stack = ExitStack()
        self.P = None
        self.nbuf = 0

    def dram(self, name, shape, dt, kind):
        return self.nc.dram_tensor(name, list(shape), dt, kind=kind).ap()

    def start(self):
        self.P = Prog(self.nc, self.stack)
        self.block = self.stack.enter_context(self.nc.Block())

    def sb(self, name, shape, dt, stack=None):
        t = (stack or self.stack).enter_context(self.nc.sbuf_tensor("sb_" + name, list(shape), dt))
        return t, Buf(name)

    def ps(self, name, shape, dt):
        t = self.stack.enter_context(self.nc.psum_tensor("ps_" + name, list(shape), dt))
        return t, Buf(name)


def run_prog(cx, body, in_maps):
    nc = cx.nc

    @cx.block.vector
    def _(v):
        body()
        cx.P.finish()

    cx.stack.close()
    res = run_bass_kernel_spmd(nc, in_maps, core_ids=list(range(NCORES)))
    return res.results


def stage_mod(inputs):
    cx = Ctx()
    nc = cx.nc
    cs = cx.dram("cs", [2, D], F32, "ExternalInput")
    mod_w = cx.dram("mod_w", [2, D, 6 * D], F32, "ExternalInput")
    mod_b = cx.dram("mod_b", [2, 6 * D], F32, "ExternalInput")
    nmix = cx.dram("norm_mix", [2, D], F32, "ExternalInput")
    nffn = cx.dram("norm_ffn", [2, D], F32, "ExternalInput")
    modv = cx.dram("modv", [2, 8, D], F32, "ExternalOutput")
    cx.start()
    P = cx.P
    NBLK = 24
    cst, cs_b = cx.sb("cst", [128, 2, 16], F32)
    scs, scs_b = cx.sb("scs", [128, 16, 2], F32)
    sg, sg_b = cx.sb("sg", [128, 2, 16], F32)
    wb = [cx.sb("wb%d" % i, [128, 16, 512], F32) for i in range(2)]
    ml, ml_b = cx.sb("ml", [2, 6 * D], F32)
    mb, mb_b = cx.sb("mb", [2, 6 * D], F32)
    gm, gm_b = cx.sb("gm", [2, D], F32)
    gf, gf_b = cx.sb("gf", [2, D], F32)
    ps = [cx.ps("ps%d" % i, [128, 512], F32) for i in range(4)]

    def body():
        for r in range(2):
            P.dma("sp", cst[:, r, :], cs[r].rearrange("(p k) -> p k", k=16), cs_b, writes=[cs_b])
        P.op("act", lambda e: e.activation(out=sg[:], in_=cst[:], func=AF.Sigmoid), reads=[cs_b], writes=[sg_b])
        P.op("dve", lambda e: e.tensor_tensor(out=scs[:].rearrange("p k r -> p r k"), in0=cst[:], in1=sg[:], op=ALU.mult),
             reads=[cs_b, sg_b], writes=[scs_b])
        for l in range(2):
            P.dma("sp", mb[:], mod_b[l:l + 1, :].broadcast_to([2, 6 * D]), mb_b, writes=[mb_b])
            P.dma("sp", gm[:], nmix[l:l + 1, :].broadcast_to([2, D]), gm_b, writes=[gm_b])
            P.dma("sp", gf[:], nffn[l:l + 1, :].broadcast_to([2, D]), gf_b, writes=[gf_b])
            for nb in range(NBLK):
                i = l * NBLK + nb
                wt, wt_b = wb[i % 2]
                pt, pt_b = ps[i % 4]
                src = mod_w[l, :, nb * 512:(nb + 1) * 512].rearrange("(p k) n -> p k n", k=16)
                q = "sp" if i % 2 == 0 else "act"
                P.dma(q, wt[:, 0:8, :], src[:, 0:8, :], wt_b, writes=[wt_b])
                P.dma(q, wt[:, 8:16, :], src[:, 8:16, :], wt_b, writes=[wt_b])
                for k in range(16):
                    P.op("pe", lambda e, k=k: e.matmul(pt[0:2, :], lhsT=scs[:, k, :], rhs=wt[:, k, :],
                                                         start=(k == 0), stop=(k == 15)),
                         reads=[scs_b, wt_b], writes=[pt_b], sig=(k == 15))
                P.op("dve", lambda e: e.tensor_tensor(out=ml[:, nb * 512:(nb + 1) * 512], in0=pt[0:2, :],
                                                      in1=mb[:, nb * 512:(nb + 1) * 512], op=ALU.add),
                     reads=[pt_b, mb_b], writes=[ml_b])
            P.op("dve", lambda e: e.scalar_tensor_tensor(out=ml[:, D:2 * D], in0=ml[:, D:2 * D], scalar=1.0, in1=gm[:],
                                                         op0=ALU.add, op1=ALU.mult),
                 reads=[ml_b, gm_b], writes=[ml_b])
            P.op("dve", lambda e: e.scalar_tensor_tensor(out=ml[:, 4 * D:5 * D], in0=ml[:, 4 * D:5 * D], scalar=1.0, in1=gf[:],
                                                         op0=ALU.add, op1=ALU.mult),
                 reads=[ml_b, gf_b], writes=[ml_b])
            order = [1, 0, 2, 4, 3, 5]
            for r, s in enumerate(order):
                P.dma("sp", modv[l, r:r + 1, :], ml[0:1, s * D:(s + 1) * D], ml_b, reads=[ml_b])
            for r, s in enumerate(order[:2]):
                P.dma("sp", modv[l, 6 + r:7 + r, :], ml[1:2, s * D:(s + 1) * D], ml_b, reads=[ml_b])

    in_maps = []
    for c in range(NCORES):
        b = c // 2
        in_maps.append({
            "cs": np.ascontiguousarray(np.stack([inputs["c"][b], inputs["c_ctx"]])),
            "mod_w": inputs["mod_w"], "mod_b": inputs["mod_b"],
            "norm_mix": inputs["norm_mix"], "norm_ffn": inputs["norm_ffn"],
        })
    res = run_prog(cx, body, in_maps)
    return [r["modv"] for r in res]


class Rot:
    def __init__(self, items):
        self.items = items
        self.i = 0

    def next(self):
        it = self.items[self.i % len(self.items)]
        self.i += 1
        return it


def rope_tables_np(pos_row, pos_col, rot):
    Q = rot // 4
    inv = (10000.0 ** (-np.arange(Q, dtype=np.float32) / Q)).astype(np.float32)
    n = pos_row.shape[0]
    cos = np.zeros((n, 2, 2, Q), np.float32)
    sin = np.zeros((n, 2, 2, Q), np.float32)
    for a, pos in enumerate((pos_row, pos_col)):
        ang = (pos.astype(np.float32)[:, None] * inv[None, :]).astype(np.float32)
        cos[:, a, :, :] = np.cos(ang)[:, None, :]
        sin[:, a, :, :] = np.sin(ang)[:, None, :]
    return cos.reshape(n, rot), sin.reshape(n, rot)


def emit_rmsnorm_stats(P, src, src_b, n, junk, junk_b, ss, ss_b, sd, sd_b, rstd, rstd_b, eps, eps_b):
    P.op("act", lambda e: e.activation(out=junk, in_=src, func=AF.Square, accum_out=ss), reads=[src_b], writes=[junk_b, ss_b])
    P.op("act", lambda e: e.activation(out=sd, in_=ss, func=AF.Sqrt, scale=1.0 / n, bias=eps), reads=[ss_b, eps_b], writes=[sd_b])
    P.op("dve", lambda e: e.reciprocal(out=rstd, in_=sd), reads=[sd_b], writes=[rstd_b])


def emit_rope(P, src, src_b, H, rot, cos, sin, tab_b, t1, t2, t_b, dst, dst_b):
    W = H * rot
    Q = rot // 4
    sv = src.rearrange("p (h f) -> p h f", h=H) if H > 1 else src
    cb = cos.unsqueeze(1).broadcast_to([128, H, rot]) if H > 1 else cos
    sb_ = sin.unsqueeze(1).broadcast_to([128, H, rot]) if H > 1 else sin
    t1v = t1[:, 0:W].rearrange("p (h f) -> p h f", h=H) if H > 1 else t1[:, 0:W]
    t2v = t2[:, 0:W].rearrange("p (h f) -> p h f", h=H) if H > 1 else t2[:, 0:W]
    P.op("dve", lambda e: e.tensor_tensor(out=t1v, in0=sv, in1=cb, op=ALU.mult), reads=[src_b, tab_b], writes=[t_b])
    P.op("dve", lambda e: e.tensor_tensor(out=t2v, in0=sv, in1=sb_, op=ALU.mult), reads=[src_b, tab_b], writes=[t_b])
    a = t1[:, 0:W].rearrange("p (g t q) -> p g t q", t=2, q=Q)
    b = t2[:, 0:W].rearrange("p (g t q) -> p g t q", t=2, q=Q)
    o = dst.rearrange("p (g t q) -> p g t q", t=2, q=Q)
    P.op("pool", lambda e: e.tensor_tensor(out=o[:, :, 0, :], in0=a[:, :, 0, :], in1=b[:, :, 1, :], op=ALU.subtract),
         reads=[t_b], writes=[dst_b])
    P.op("pool", lambda e: e.tensor_tensor(out=o[:, :, 1, :], in0=a[:, :, 1, :], in1=b[:, :, 0, :], op=ALU.add),
         reads=[t_b], writes=[dst_b])


def emit_tgroup(P, tps, ident, id_b, srcs, src_b, f, dst, dst_b, eng="act"):
    n = len(srcs)
    assert n <= 8
    tp, tp_b = tps.next()
    for j, s in enumerate(srcs):
        P.op("pe", lambda e, j=j, s=s: e.transpose(out=tp[0:f, j * 128:(j + 1) * 128], in_=s, identity=ident),
             reads=[src_b, id_b], writes=[tp_b], sig=(j == n - 1))
    src_v = tp[0:f, 0:n * 128].rearrange("p (n t) -> p n t", t=128)
    if eng == "act":
        P.op("act", lambda e: e.copy(out=dst, in_=src_v), reads=[tp_b], writes=[dst_b])
    else:
        P.op("dve", lambda e: e.tensor_copy(out=dst, in_=src_v), reads=[tp_b], writes=[dst_b])


NT2 = 17
TT2 = NT2 * 128


def stage_proj0(inputs, modv):
    cx = Ctx()
    nc = cx.nc
    xin = cx.dram("xin", [NT2, 128, D], F32, "ExternalInput")
    mv = cx.dram("mv", [8, D], F32, "ExternalInput")
    w_in = cx.dram("w_in", [D, 4160], F32, "ExternalInput")
    qn = cx.dram("qn", [1, 512], F32, "ExternalInput")
    kvn = cx.dram("kvn", [1, 512], F32, "ExternalInput")
    w_uq = cx.dram("w_uq", [512, 1536], F32, "ExternalInput")
    w_ukv = cx.dram("w_ukv", [512, 2048], F32, "ExternalInput")
    rope = cx.dram("rope", [NT2, 128, 384], F32, "ExternalInput")
    identd = cx.dram("ident", [128, 128], F32, "ExternalInput")
    qnT = cx.dram("qnT", [8, 128, TT2], BF16, "ExternalOutput")
    qrT = cx.dram("qrT", [8, 64, TT2], BF16, "ExternalOutput")
    qdT = cx.dram("qdT", [8, 128, TT2], BF16, "ExternalOutput")
    knT = cx.dram("knT", [8, 128, TT2], BF16, "ExternalOutput")
    krT = cx.dram("krT", [64, TT2], BF16, "ExternalOutput")
    kdT = cx.dram("kdT", [8, 128, TT2], BF16, "ExternalOutput")
    va = cx.dram("va", [TT2, 1024], BF16, "ExternalOutput")
    vd = cx.dram("vd", [TT2, 1024], BF16, "ExternalOutput")
    cx.start()
    P = cx.P
    wA, wA_b = cx.sb("wA", [128, 16, 2112], BF16)
    wq, wq_b = cx.sb("wq", [128, 4, 1536], BF16)
    wkv, wkv_b = cx.sb("wkv", [128, 4, 2048], BF16)
    ident, id_b = cx.sb("identb", [128, 128], BF16)
    A1, A1_b = cx.sb("A1", [128, D], F32)
    B1, B1_b = cx.sb("B1", [128, D], F32)
    qnt, qnt_b = cx.sb("qnt", [128, 512], F32)
    kvnt, kvnt_b = cx.sb("kvnt", [128, 512], F32)
    eps, eps_b = cx.sb("eps", [128, 1], F32)
    xts = Rot([cx.sb("xt%d" % i, [128, D], F32) for i in range(2)])
    rts = Rot([cx.sb("rt%d" % i, [128, 384], F32) for i in range(2)])
    hb, hb_b = cx.sb("hb", [128, D], BF16)
    hT, hT_b = cx.sb("hT", [128, 16, 128], BF16)
    sm, sm_b = cx.sb("sm", [128, 8], F32)
    cn, cn_b = cx.sb("cn", [128, 512], BF16)
    cnT, cnT_b = cx.sb("cnT", [128, 4, 128], BF16)
    t1, t12_b = cx.sb("t1", [128, 1024], F32)
    t2, _ = cx.sb("t2", [128, 1024], F32)
    tok, tok_b = cx.sb("tok", [128, 1024], BF16)
    tok2, tok2_b = cx.sb("tok2", [128, 1024], BF16)
    vst = Rot([cx.sb("vst%d" % i, [128, 1024], BF16) for i in range(2)])
    st128 = Rot([cx.sb("st128_%d" % i, [128, 8, 128], BF16) for i in range(3)])
    st64 = Rot([cx.sb("st64_%d" % i, [64, 8, 128], BF16) for i in range(2)])
    tps = Rot([cx.ps("tp%d" % i, [128, 1024], BF16) for i in range(2)])
    pjs = Rot([cx.ps("pj%d" % i, [128, 512], F32) for i in range(6)])

    def load_w(dst, dst_b, src, ncols, kch):
        v = src.rearrange("(k p) n -> p k n", p=128)
        for c0 in range(0, ncols, 512):
            c1 = min(ncols, c0 + 512)
            P.dma("pool", dst[:, :, c0:c1], v[:, :, c0:c1], dst_b, writes=[dst_b])

    def mm(lhsT_of_k, l_b, rhs_of_k, r_b, nk, n):
        pj, pj_b = pjs.next()
        for k in range(nk):
            P.op("pe", lambda e, k=k: e.matmul(pj[:, 0:n], lhsT=lhsT_of_k(k), rhs=rhs_of_k(k), start=(k == 0), stop=(k == nk - 1)),
                 reads=[l_b, r_b], writes=[pj_b], sig=(k == nk - 1))
        return pj, pj_b

    def front(i, a_row, b_row):
        xt, xt_b = xts.next()
        rt, rt_b = rts.next()
        P.dma("sp", xt[:], xin[i], xt_b, writes=[xt_b])
        P.dma("sp", rt[:], rope[i], rt_b, writes=[rt_b])
        if a_row is not None:
            P.dma("sp", A1[:], mv[a_row:a_row + 1, :].broadcast_to([128, D]), A1_b, writes=[A1_b])
            P.dma("sp", B1[:], mv[b_row:b_row + 1, :].broadcast_to([128, D]), B1_b, writes=[B1_b])
        emit_rmsnorm_stats(P, xt[:], xt_b, D, hb[:], hb_b, sm[:, 0:1], sm_b, sm[:, 1:2], sm_b, sm[:, 2:3], sm_b, eps[:], eps_b)
        P.op("dve", lambda e: e.scalar_tensor_tensor(out=xt[:], in0=xt[:], scalar=sm[:, 2:3], in1=A1[:], op0=ALU.mult, op1=ALU.mult),
             reads=[xt_b, sm_b, A1_b], writes=[xt_b])
        P.op("dve", lambda e: e.tensor_tensor(out=hb[:], in0=xt[:], in1=B1[:], op=ALU.add), reads=[xt_b, B1_b], writes=[hb_b])
        for g in range(2):
            emit_tgroup(P, tps, ident[:], id_b, [hb[:, (g * 8 + j) * 128:(g * 8 + j + 1) * 128] for j in range(8)], hb_b, 128,
                        hT[:, g * 8:(g + 1) * 8, :], hT_b, eng="act")
        return rt, rt_b

    def lowrank(i, c0, gt, g_b, wexp, wexp_b, nblk):
        pj, pj_b = mm(lambda k: hT[:, k, :], hT_b, lambda k: wA[:, k, c0:c0 + 512], wA_b, 16, 512)
        emit_rmsnorm_stats(P, pj[:], pj_b, 512, cn[:], cn_b, sm[:, 3:4], sm_b, sm[:, 4:5], sm_b, sm[:, 5:6], sm_b, eps[:], eps_b)
        P.op("dve", lambda e: e.scalar_tensor_tensor(out=cn[:], in0=pj[:], scalar=sm[:, 5:6], in1=gt[:], op0=ALU.mult, op1=ALU.mult),
             reads=[pj_b, sm_b, g_b], writes=[cn_b])
        emit_tgroup(P, tps, ident[:], id_b, [cn[:, j * 128:(j + 1) * 128] for j in range(4)], cn_b, 128, cnT[:], cnT_b, eng="act")
        outs = []
        for nb in range(nblk):
            outs.append(mm(lambda k: cnT[:, k, :], cnT_b, lambda k, nb=nb: wexp[:, k, nb * 512:(nb + 1) * 512], wexp_b, 4, 512))
        return outs

    def out_T128(i, src, src_b, dstT):
        st, st_b = st128.next()
        emit_tgroup(P, tps, ident[:], id_b, [src[:, j * 128:(j + 1) * 128] for j in range(8)], src_b, 128, st[:], st_b, eng="act")
        P.dma("sp", dstT[:, :, i * 128:(i + 1) * 128].rearrange("h p t -> p h t"), st[:], st_b, reads=[st_b])

    def body():
        P.dma("pool", ident[:], identd, id_b, writes=[id_b])
        P.op("dve", lambda e: e.memset(eps[:], EPS), writes=[eps_b])
        P.dma("sp", qnt[:], qn.broadcast_to([128, 512]), qnt_b, writes=[qnt_b])
        P.dma("sp", kvnt[:], kvn.broadcast_to([128, 512]), kvnt_b, writes=[kvnt_b])
        load_w(wq, wq_b, w_uq, 1536, 4)
        load_w(wkv, wkv_b, w_ukv, 2048, 4)
        load_w(wA, wA_b, w_in[:, 0:2112], 2112, 16)
        for i in range(NT2):
            rt, rt_b = front(i, 0 if i == 0 else (6 if i == NT2 - 1 else None), 1 if i == 0 else (7 if i == NT2 - 1 else None))
            cos64, sin64, cos128, sin128 = rt[:, 0:64], rt[:, 64:128], rt[:, 128:256], rt[:, 256:384]
            qb = lowrank(i, 0, qnt, qnt_b, wq, wq_b, 3)
            for j in range(2):
                P.op("act", lambda e, j=j: e.copy(out=tok[:, j * 512:(j + 1) * 512], in_=qb[j][0][:]), reads=[qb[j][1]], writes=[tok_b])
            out_T128(i, tok, tok_b, qnT)
            emit_rope(P, qb[2][0][:], qb[2][1], 8, 64, cos64, sin64, rt_b, t1, t2, t12_b, tok2[:, 0:512], tok2_b)
            st, st_b = st64.next()
            emit_tgroup(P, tps, ident[:], id_b, [tok2[:, j * 64:(j + 1) * 64] for j in range(8)], tok2_b, 64, st[:], st_b, eng="act")
            P.dma("sp", qrT[:, :, i * 128:(i + 1) * 128].rearrange("h p t -> p h t"), st[:], st_b, reads=[st_b])
            kb = lowrank(i, 512, kvnt, kvnt_b, wkv, wkv_b, 4)
            for j in range(2):
                P.op("act", lambda e, j=j: e.copy(out=tok[:, j * 512:(j + 1) * 512], in_=kb[j][0][:]), reads=[kb[j][1]], writes=[tok_b])
            out_T128(i, tok, tok_b, knT)
            vs, vs_b = vst.next()
            for j in range(2):
                P.op("act", lambda e, j=j: e.copy(out=vs[:, j * 512:(j + 1) * 512], in_=kb[2 + j][0][:]), reads=[kb[2 + j][1]], writes=[vs_b])
            P.dma("sp", va[i * 128:(i + 1) * 128, :], vs[:], vs_b, reads=[vs_b])
            pj, pj_b = mm(lambda k: hT[:, k, :], hT_b, lambda k: wA[:, k, 1024:1088], wA_b, 16, 64)
            emit_rope(P, pj[:, 0:64], pj_b, 1, 64, cos64, sin64, rt_b, t1, t2, t12_b, tok2[:, 0:64], tok2_b)
            st, st_b = st64.next()
            emit_tgroup(P, tps, ident[:], id_b, [tok2[:, 0:64]], tok2_b, 64, st[:, 0:1, :], st_b, eng="act")
            P.dma("sp", krT[:, i * 128:(i + 1) * 128], st[:, 0, :], st_b, reads=[st_b])
            for j in range(2):
                pj, pj_b = mm(lambda k: hT[:, k, :], hT_b, lambda k, j=j: wA[:, k, 1088 + j * 512:1088 + (j + 1) * 512], wA_b, 16, 512)
                emit_rope(P, pj[:], pj_b, 4, 128, cos128, sin128, rt_b, t1, t2, t12_b, tok[:, j * 512:(j + 1) * 512], tok_b)
            out_T128(i, tok, tok_b, qdT)
        load_w(wA, wA_b, w_in[:, 2112:4160], 2048, 16)
        for i in range(NT2):
            rt, rt_b = front(i, 0 if i == 0 else (6 if i == NT2 - 1 else None), 1 if i == 0 else (7 if i == NT2 - 1 else None))
            cos128, sin128 = rt[:, 128:256], rt[:, 256:384]
            for j in range(2):
                pj, pj_b = mm(lambda k: hT[:, k, :], hT_b, lambda k, j=j: wA[:, k, j * 512:(j + 1) * 512], wA_b, 16, 512)
                emit_rope(P, pj[:], pj_b, 4, 128, cos128, sin128, rt_b, t1, t2, t12_b, tok[:, j * 512:(j + 1) * 512], tok_b)
            out_T128(i, tok, tok_b, kdT)
            vs, vs_b = vst.next()
            for j in range(2):
                pj, pj_b = mm(lambda k: hT[:, k, :], hT_b, lambda k, j=j: wA[:, k, 1024 + j * 512:1024 + (j + 1) * 512], wA_b, 16, 512)
                P.op("act", lambda e, j=j: e.copy(out=vs[:, j * 512:(j + 1) * 512], in_=pj[:]), reads=[pj_b], writes=[vs_b])
            P.dma("sp", vd[i * 128:(i + 1) * 128, :], vs[:], vs_b, reads=[vs_b])

    j0 = 0
    w_uq_p = np.ascontiguousarray(np.concatenate([
        inputs["mla_w_uq"][j0].reshape(512, 8, 192)[:, :, :128].reshape(512, 1024),
        inputs["mla_w_uq"][j0].reshape(512, 8, 192)[:, :, 128:].reshape(512, 512)], axis=1))
    w_ukv_p = np.ascontiguousarray(np.concatenate([
        inputs["mla_w_ukv"][j0].reshape(512, 8, 256)[:, :, :128].reshape(512, 1024),
        inputs["mla_w_ukv"][j0].reshape(512, 8, 256)[:, :, 128:].reshape(512, 1024)], axis=1))
    ident_np = np.eye(128, dtype=np.float32)
    in_maps = []
    for c in range(NCORES):
        b, hf = c // 2, c % 2
        xs = inputs["x"][b, hf * TOWN:(hf + 1) * TOWN].reshape(16, 128, D)
        cs_ = inputs["ctx"][b, hf * 128:(hf + 1) * 128][None]
        t = np.arange(hf * TOWN, (hf + 1) * TOWN)
        c64, s64 = rope_tables_np(t // 64, t % 64, 64)
        c128, s128 = rope_tables_np(t // 64, t % 64, 128)
        rp = np.concatenate([c64, s64, c128, s128], axis=1).reshape(16, 128, 384)
        rc = np.concatenate([np.ones((128, 64), np.float32), np.zeros((128, 64), np.float32),
                             np.ones((128, 128), np.float32), np.zeros((128, 128), np.float32)], axis=1)[None]
        in_maps.append({
            "xin": np.ascontiguousarray(np.concatenate([xs, cs_], axis=0)),
            "mv": np.ascontiguousarray(modv[c][0]),
            "w_in": inputs["even_w_in"][j0], "qn": inputs["mla_q_norm"][j0][None], "kvn": inputs["mla_kv_norm"][j0][None],
            "w_uq": w_uq_p, "w_ukv": w_ukv_p,
            "rope": np.ascontiguousarray(np.concatenate([rp, rc], axis=0).astype(np.float32)),
            "ident": ident_np,
        })
    return run_prog(cx, body, in_maps)


NKEY = SEQ + CTX
NKC = NKEY // 128


def stage_attn0(inputs, modv, s2):
    cx = Ctx()
    nc = cx.nc
    qnT = cx.dram("qnT", [8, 128, TOWN], BF16, "ExternalInput")
    qrT = cx.dram("qrT", [8, 64, TOWN], BF16, "ExternalInput")
    qdT = cx.dram("qdT", [8, 128, TOWN], BF16, "ExternalInput")
    knT = cx.dram("knT", [8, 128, NKEY], BF16, "ExternalInput")
    krT = cx.dram("krT", [64, NKEY], BF16, "ExternalInput")
    kdT = cx.dram("kdT", [8, 128, NKEY], BF16, "ExternalInput")
    va = cx.dram("va", [NKEY, 1024], BF16, "ExternalInput")
    vd = cx.dram("vd", [NKEY, 1024], BF16, "ExternalInput")
    xin = cx.dram("xin", [16, 128, D], F32, "ExternalInput")
    mv = cx.dram("mv", [8, D], F32, "ExternalInput")
    lamp = cx.dram("lamp", [4, 128], F32, "ExternalInput")
    subln = cx.dram("subln", [256], F32, "ExternalInput")
    w_out = cx.dram("w_out", [D, D], F32, "ExternalInput")
    xout = cx.dram("xout", [16, 128, D], F32, "ExternalOutput")
    cx.start()
    P = cx.P
    LAM_INIT = 0.8 - 0.6 * math.exp(-0.3 * 0)
    catT, cat_b = cx.sb("catT", [128, 16, TOWN], BF16)
    eps, eps_b = cx.sb("eps", [128, 1], F32)
    lt, lt_b = cx.sb("lt", [128, 4, 128], F32)
    lsm, lsm_b = cx.sb("lsm", [128, 8], F32)
    sls, sls_b = cx.sb("sls", [128, 2], F32)
    pbanks = [cx.ps("pb%d" % i, [128, 512], F32) for i in range(8)]
    pS = Rot(pbanks[0:3])
    pO = pbanks[3:5]
    pD, pD_b = pbanks[5]
    pX = Rot(pbanks[6:8])
    phA = ExitStack()
    Ks = Rot([cx.sb("K%d" % i, [128, NKEY], BF16, phA) for i in range(4)])
    Kr, Kr_b = cx.sb("Kr", [64, NKEY], BF16, phA)
    Vs = Rot([cx.sb("V%d" % i, [128, NKC, 256], BF16, phA) for i in range(2)])
    Qs = Rot([cx.sb("Q%d" % i, [128, TOWN], BF16, phA) for i in range(4)])
    pts = Rot([cx.sb("pt%d" % i, [128, 512], BF16, phA) for i in range(3)])
    ones_b, ones_bb = cx.sb("ones_b", [128, 128], BF16, phA)
    ones_f, ones_fb = cx.sb("ones_f", [128, 128], F32, phA)
    rd, rd_b = cx.sb("rd", [128, 512], F32, phA)
    o0 = [cx.sb("o0_%d" % i, [128, 512], F32, phA) for i in range(2)]
    o1, o1_b = cx.sb("o1", [128, 512], F32, phA)
    sq, sq_b = cx.sb("sq", [128, 512], F32, phA)
    rs, rs_b = cx.sb("rs", [128, 512], F32, phA)

    def attn_unit(pairs, V, V_b, ndv, scale, qb):
        qs = slice(qb * 512, (qb + 1) * 512)

        def emit_S(kc):
            ps_, ps_b = pS.next()
            for j, (K, K_b, Q, Q_b) in enumerate(pairs):
                P.op("pe", lambda e, j=j, K=K, Q=Q: e.matmul(ps_[:], lhsT=K[:, kc * 128:(kc + 1) * 128], rhs=Q[:, qs],
                                                          start=(j == 0), stop=(j == len(pairs) - 1)),
                     reads=[K_b, Q_b], writes=[ps_b], sig=(j == len(pairs) - 1))
            pt, pt_b = pts.next()
            P.op("act", lambda e: e.activation(out=pt[:], in_=ps_[:], func=AF.Exp, scale=scale), reads=[ps_b], writes=[pt_b])
            return pt, pt_b

        def emit_PV(kc, pt, pt_b):
            for dc in range(ndv):
                P.op("pe", lambda e, dc=dc: e.matmul(pO[dc][0][:], lhsT=V[:, kc, dc * 128:(dc + 1) * 128], rhs=pt[:],
                                                     start=(kc == 0), stop=(kc == NKC - 1)),
                     reads=[V_b, pt_b], writes=[pO[dc][1]], sig=False)
            P.op("pe", lambda e: e.matmul(pD[:], lhsT=ones_b[:], rhs=pt[:], start=(kc == 0), stop=(kc == NKC - 1)),
                 reads=[ones_bb, pt_b], writes=[pD_b] + [pO[dc][1] for dc in range(ndv)], sig=True)

        pend = [emit_S(0)]
        for kc in range(NKC):
            if kc + 1 < NKC:
                pend.append(emit_S(kc + 1))
            pt, pt_b = pend.pop(0)
            emit_PV(kc, pt, pt_b)
        P.op("dve", lambda e: e.reciprocal(out=rd[:], in_=pD[:]), reads=[pD_b], writes=[rd_b])

    def body():
        P.op("dve", lambda e: e.memset(eps[:], EPS), writes=[eps_b])
        P.op("dve", lambda e: e.memset(ones_b[:], 1.0), writes=[ones_bb])
        P.op("dve", lambda e: e.memset(ones_f[:], 1.0), writes=[ones_fb])
        for r in range(4):
            P.dma("sp", lt[:, r, :], lamp[r:r + 1, :].broadcast_to([128, 128]), lt_b, writes=[lt_b])
        for r in range(2):
            P.op("dve", lambda e, r=r: e.tensor_tensor(out=lt[:, 2 * r, :], in0=lt[:, 2 * r, :], in1=lt[:, 2 * r + 1, :], op=ALU.mult),
                 reads=[lt_b], writes=[lt_b])
            P.op("dve", lambda e, r=r: e.reduce_sum(out=lsm[:, r:r + 1], in_=lt[:, 2 * r, :], axis=AX.X), reads=[lt_b], writes=[lsm_b])
        P.op("act", lambda e: e.activation(out=lsm[:, 2:4], in_=lsm[:, 0:2], func=AF.Exp), reads=[lsm_b], writes=[lsm_b])
        P.op("dve", lambda e: e.tensor_tensor(out=lsm[:, 4:5], in0=lsm[:, 3:4], in1=lsm[:, 2:3], op=ALU.subtract), reads=[lsm_b], writes=[lsm_b])
        P.op("dve", lambda e: e.tensor_scalar(out=lsm[:, 5:6], in0=lsm[:, 4:5], scalar1=-LAM_INIT, scalar2=None, op0=ALU.add),
             reads=[lsm_b], writes=[lsm_b])
        neglam = lsm[:, 5:6]
        for c in range(2):
            P.dma("sp", sls[:, c:c + 1], subln[c * 128:(c + 1) * 128].rearrange("(p o) -> p o", o=1), sls_b, writes=[sls_b])
        P.op("dve", lambda e: e.tensor_scalar(out=sls[:], in0=sls[:], scalar1=1.0 - LAM_INIT, scalar2=None, op0=ALU.mult),
             reads=[sls_b], writes=[sls_b])
        P.dma("sp", Kr[:], krT, Kr_b, writes=[Kr_b])
        sc_a = 1.0 / math.sqrt(192.0)
        for hh in range(8):
            K, K_b = Ks.next()
            V, V_b = Vs.next()
            Qn, Qn_b = Qs.next()
            Qr, Qr_b = Qs.next()
            P.dma("sp", K[:], knT[hh], K_b, writes=[K_b])
            P.dma("sp", V[:, :, 0:128], va[:, hh * 128:(hh + 1) * 128].rearrange("(c p) d -> p c d", p=128), V_b, writes=[V_b])
            P.dma("sp", Qn[:], qnT[hh], Qn_b, writes=[Qn_b])
            P.dma("sp", Qr[0:64, :], qrT[hh], Qr_b, writes=[Qr_b])
            for qb in range(4):
                attn_unit([(K, K_b, Qn, Qn_b), (Kr[:], Kr_b, Qr[0:64, :], Qr_b)], V, V_b, 1, sc_a, qb)
                P.op("dve", lambda e: e.tensor_tensor(out=catT[:, hh, qb * 512:(qb + 1) * 512], in0=pO[0][0][:], in1=rd[:], op=ALU.mult),
                     reads=[pO[0][1], rd_b], writes=[cat_b])
        sc_d = 1.0 / math.sqrt(128.0)
        for hd in range(4):
            V, V_b = Vs.next()
            P.dma("sp", V[:], vd[:, hd * 256:(hd + 1) * 256].rearrange("(c p) d -> p c d", p=128), V_b, writes=[V_b])
            KQ = []
            for m in range(2):
                K, K_b = Ks.next()
                Q, Q_b = Qs.next()
                P.dma("sp", K[:], kdT[m * 4 + hd], K_b, writes=[K_b])
                P.dma("sp", Q[:], qdT[m * 4 + hd], Q_b, writes=[Q_b])
                KQ.append((K, K_b, Q, Q_b))
            for qb in range(4):
                attn_unit([KQ[0]], V, V_b, 2, sc_d, qb)
                for dc in range(2):
                    P.op("dve", lambda e, dc=dc: e.tensor_tensor(out=o0[dc][0][:], in0=pO[dc][0][:], in1=rd[:], op=ALU.mult),
                         reads=[pO[dc][1], rd_b], writes=[o0[dc][1]])
                attn_unit([KQ[1]], V, V_b, 2, sc_d, qb)
                px, px_b = pX.next()
                for dc in range(2):
                    P.op("dve", lambda e, dc=dc: e.tensor_tensor(out=o1[:], in0=pO[dc][0][:], in1=rd[:], op=ALU.mult),
                         reads=[pO[dc][1], rd_b], writes=[o1_b])
                    P.op("dve", lambda e, dc=dc: e.scalar_tensor_tensor(out=o0[dc][0][:], in0=o1[:], scalar=neglam, in1=o0[dc][0][:],
                                                                        op0=ALU.mult, op1=ALU.add),
                         reads=[o1_b, lsm_b, o0[dc][1]], writes=[o0[dc][1]])
                    P.op("pool", lambda e, dc=dc: e.tensor_tensor(out=sq[:], in0=o0[dc][0][:], in1=o0[dc][0][:], op=ALU.mult),
                         reads=[o0[dc][1]], writes=[sq_b])
                    P.op("pe", lambda e, dc=dc: e.matmul(px[:], lhsT=ones_f[:], rhs=sq[:], start=(dc == 0), stop=(dc == 1)),
                         reads=[ones_fb, sq_b], writes=[px_b], sig=True)
                P.op("act", lambda e: e.activation(out=rs[:], in_=px[:], func=AF.Sqrt, scale=1.0 / 256.0, bias=eps[:]),
                     reads=[px_b, eps_b], writes=[rs_b])
                P.op("dve", lambda e: e.reciprocal(out=rs[:], in_=rs[:]), reads=[rs_b], writes=[rs_b])
                for dc in range(2):
                    P.op("dve", lambda e, dc=dc: e.scalar_tensor_tensor(out=catT[:, 8 + hd * 2 + dc, qb * 512:(qb + 1) * 512], in0=o0[dc][0][:],
                                                                        scalar=sls[:, dc:dc + 1], in1=rs[:], op0=ALU.mult, op1=ALU.mult),
                         reads=[o0[dc][1], sls_b, rs_b], writes=[cat_b])
        P.barrier()
        phA.close()
        phB = ExitStack()
        wo, wo_b = cx.sb("wo", [128, 16, D], BF16, phB)
        G1, G1_b = cx.sb("G1", [128, D], F32, phB)
        xts = Rot([cx.sb("xt%d" % i, [128, D], F32, phB) for i in range(2)])
        yts = Rot([cx.sb("yt%d" % i, [128, D], F32, phB) for i in range(2)])
        wv = w_out.rearrange("(k p) n -> p k n", p=128)
        for c0 in range(0, D, 512):
            P.dma("pool", wo[:, :, c0:c0 + 512], wv[:, :, c0:c0 + 512], wo_b, writes=[wo_b])
        P.dma("sp", G1[:], mv[2:3, :].broadcast_to([128, D]), G1_b, writes=[G1_b])
        for tt in range(16):
            xt, xt_b = xts.next()
            yt, yt_b = yts.next()
            P.dma("sp", xt[:], xin[tt], xt_b, writes=[xt_b])
            for nb in range(4):
                pj, pj_b = pX.next() if nb % 2 == 0 else pS.next()
                for f in range(16):
                    P.op("pe", lambda e, f=f: e.matmul(pj[:], lhsT=catT[:, f, tt * 128:(tt + 1) * 128], rhs=wo[:, f, nb * 512:(nb + 1) * 512],
                                                       start=(f == 0), stop=(f == 15)),
                         reads=[cat_b, wo_b], writes=[pj_b], sig=(f == 15))
                cs = slice(nb * 512, (nb + 1) * 512)
                P.op("dve", lambda e: e.tensor_tensor(out=yt[:, cs], in0=pj[:], in1=G1[:, cs], op=ALU.mult), reads=[pj_b, G1_b], writes=[yt_b])
                P.op("pool", lambda e: e.tensor_tensor(out=yt[:, cs], in0=yt[:, cs], in1=xt[:, cs], op=ALU.add), reads=[yt_b, xt_b], writes=[yt_b])
            P.dma("sp", xout[tt], yt[:], yt_b, reads=[yt_b])
        P.finish()
        phB.close()

    j0 = 0
    lam_np = np.ascontiguousarray(np.stack([inputs["diff_lambda_q1"][j0], inputs["diff_lambda_k1"][j0],
                                            inputs["diff_lambda_q2"][j0], inputs["diff_lambda_k2"][j0]]))
    in_maps = []
    for c in range(NCORES):
        b, hf = c // 2, c % 2
        c0, c1 = 2 * b, 2 * b + 1

        def fullk(name, ax):
            a0, a1 = s2[c0][name], s2[c1][name]
            sl = [slice(None)] * a0.ndim
            own, cx_ = list(sl), list(sl)
            own[ax] = slice(0, TOWN)
            cx_[ax] = slice(TOWN, TT2)
            return np.ascontiguousarray(np.concatenate([a0[tuple(own)], a1[tuple(own)], a0[tuple(cx_)], a1[tuple(cx_)]], axis=ax))

        in_maps.append({
            "qnT": np.ascontiguousarray(s2[c]["qnT"][:, :, :TOWN]), "qrT": np.ascontiguousarray(s2[c]["qrT"][:, :, :TOWN]),
            "qdT": np.ascontiguousarray(s2[c]["qdT"][:, :, :TOWN]),
            "knT": fullk("knT", 2), "krT": fullk("krT", 1), "kdT": fullk("kdT", 2), "va": fullk("va", 0), "vd": fullk("vd", 0),
            "xin": np.ascontiguousarray(inputs["x"][b, hf * TOWN:(hf + 1) * TOWN].reshape(16, 128, D)),
            "mv": np.ascontiguousarray(modv[c][0]), "lamp": lam_np, "subln": inputs["diff_subln"][j0],
            "w_out": inputs["even_w_out"][j0],
        })
    res = run_prog(cx, body, in_maps)
    return [r["xout"].reshape(TOWN, D) for r in res]


CAP = 512
NEL = 8


def stage_ffn(inputs, modv, xfull, layer):
    cx = Ctx()
    nc = cx.nc
    xin = cx.dram("xin", [32, 128, D], F32, "ExternalInput")
    mv = cx.dram("mv", [8, D], F32, "ExternalInput")
    rw = cx.dram("rw", [D, 16], F32, "ExternalInput")
    wg = cx.dram("wg", [NEL, D, D], F32, "ExternalInput")
    wu = cx.dram("wu", [NEL, D, D], F32, "ExternalInput")
    wd = cx.dram("wd", [NEL, D, D], F32, "ExternalInput")
    identd = cx.dram("ident", [128, 128], F32, "ExternalInput")
    ltsd = cx.dram("lts", [128, 128], F32, "ExternalInput")
    tokidd = cx.dram("tokid", [128, 32], F32, "ExternalInput")
    iotad = cx.dram("iota", [128, 512], F32, "ExternalInput")
    acc = cx.dram("acc", [SEQ, D], F32, "ExternalOutput")
    h2d = cx.dram("h2d", [SEQ, D], BF16, "Internal")
    cx.start()
    P = cx.P
    h2d_b = Buf("h2d")
    accS = [Buf("acc%d" % i) for i in range(4)]
    identf, idf_b = cx.sb("identf", [128, 128], F32)
    identb, idb_b = cx.sb("identb", [128, 128], BF16)
    lts, lts_b = cx.sb("lts", [128, 128], F32)
    ones_f, ones_fb = cx.sb("ones_f", [128, 128], F32)
    tokid, tokid_b = cx.sb("tokid", [128, 32], F32)
    iota, iota_b = cx.sb("iota", [128, 512], F32)
    eps, eps_b = cx.sb("eps", [128, 1], F32)
    aff, aff_b = cx.sb("aff", [128, 32, 16], F32)
    mask, mask_b = cx.sb("mask", [128, 32, 16], F32)
    pos, pos_b = cx.sb("pos", [128, 32, 16], F32)
    sm, sm_b = cx.sb("sm", [128, 16], F32)
    pbanks = [cx.ps("pb%d" % i, [128, 512], F32) for i in range(6)]
    pR = Rot(pbanks)
    tps = Rot([cx.ps("tp%d" % i, [128, 1024], BF16) for i in range(2)])

    def body():
        P.dma("sp", identf[:], identd, idf_b, writes=[idf_b])
        P.dma("pool", identb[:], identd, idb_b, writes=[idb_b])
        P.dma("sp", lts[:], ltsd, lts_b, writes=[lts_b])
        P.dma("sp", tokid[:], tokidd, tokid_b, writes=[tokid_b])
        P.dma("sp", iota[:], iotad, iota_b, writes=[iota_b])
        P.op("dve", lambda e: e.memset(eps[:], EPS), writes=[eps_b])
        P.op("dve", lambda e: e.memset(ones_f[:], 1.0), writes=[ones_fb])
        phA = ExitStack()
        A2, A2_b = cx.sb("A2", [128, D], F32, phA)
        B2, B2_b = cx.sb("B2", [128, D], F32, phA)
        zt, zt_b = cx.sb("zt", [128, D], F32, phA)
        rwt, rwt_b = cx.sb("rwt", [128, 16, 16], F32, phA)
        xts = Rot([cx.sb("xt%d" % i, [128, D], F32, phA) for i in range(2)])
        hbs = Rot([cx.sb("hb%d" % i, [128, D], BF16, phA) for i in range(2)])
        hTf, hTf_b = cx.sb("hTf", [128, 16, 128], F32, phA)
        ex, ex_b = cx.sb("ex", [128, 16], F32, phA)
        affT, affT_b = cx.sb("affT", [16, SEQ], F32, phA)
        junk, junk_b = cx.sb("junk", [16, SEQ], BF16, phA)
        bs, bs_b = cx.sb("bs", [16, 16], F32, phA)
        P.dma("sp", A2[:], mv[3:4, :].broadcast_to([128, D]), A2_b, writes=[A2_b])
        P.dma("sp", B2[:], mv[4:5, :].broadcast_to([128, D]), B2_b, writes=[B2_b])
        P.dma("sp", rwt[:], rw.rearrange("(k p) e -> p k e", p=128), rwt_b, writes=[rwt_b])
        P.op("pool", lambda e: e.memset(zt[:], 0.0), writes=[zt_b])
        for i in range(32):
            P.dma("act", acc[i * 128:(i + 1) * 128, :], zt[:], zt_b, reads=[zt_b], writes=[accS[i % 4]])
        for i in range(32):
            xt, xt_b = xts.next()
            hb, hb_b = hbs.next()
            P.dma("sp", xt[:], xin[i], xt_b, writes=[xt_b])
            emit_rmsnorm_stats(P, xt[:], xt_b, D, hb[:], hb_b, sm[:, 0:1], sm_b, sm[:, 1:2], sm_b, sm[:, 2:3], sm_b, eps[:], eps_b)
            P.op("dve", lambda e: e.scalar_tensor_tensor(out=xt[:], in0=xt[:], scalar=sm[:, 2:3], in1=A2[:], op0=ALU.mult, op1=ALU.mult),
                 reads=[xt_b, sm_b, A2_b], writes=[xt_b])
            P.op("pool", lambda e: e.tensor_tensor(out=xt[:], in0=xt[:], in1=B2[:], op=ALU.add), reads=[xt_b, B2_b], writes=[xt_b])
            P.op("act", lambda e: e.copy(out=hb[:], in_=xt[:]), reads=[xt_b], writes=[hb_b])
            P.dma("sp", h2d[i * 128:(i + 1) * 128, :], hb[:], hb_b, reads=[hb_b], writes=[h2d_b])
            for g in range(4):
                pt, pt_b = pR.next()
                for j in range(4):
                    k = g * 4 + j
                    P.op("pe", lambda e, j=j, k=k: e.transpose(out=pt[:, j * 128:(j + 1) * 128], in_=xt[:, k * 128:(k + 1) * 128], identity=identf[:]),
                         reads=[xt_b, idf_b], writes=[pt_b], sig=(j == 3))
                dst = hTf[:, g * 4:(g + 1) * 4, :]
                srcv = pt[:].rearrange("p (n t) -> p n t", t=128)
                if g % 2 == 0:
                    P.op("act", lambda e: e.copy(out=dst, in_=srcv), reads=[pt_b], writes=[hTf_b])
                else:
                    P.op("dve", lambda e: e.tensor_copy(out=dst, in_=srcv), reads=[pt_b], writes=[hTf_b])
            pl, pl_b = pR.next()
            for k in range(16):
                P.op("pe", lambda e, k=k: e.matmul(pl[:, 0:16], lhsT=hTf[:, k, :], rhs=rwt[:, k, :], start=(k == 0), stop=(k == 15)),
                     reads=[hTf_b, rwt_b], writes=[pl_b], sig=(k == 15))
            P.op("dve", lambda e: e.reduce_max(out=sm[:, 3:4], in_=pl[:, 0:16], axis=AX.X), reads=[pl_b], writes=[sm_b])
            P.op("dve", lambda e: e.tensor_scalar(out=sm[:, 4:5], in0=sm[:, 3:4], scalar1=-1.0, scalar2=None, op0=ALU.mult), reads=[sm_b], writes=[sm_b])
            P.op("act", lambda e: e.activation(out=ex[:], in_=pl[:, 0:16], func=AF.Exp, bias=sm[:, 4:5], accum_out=sm[:, 5:6]),
                 reads=[pl_b, sm_b], writes=[ex_b, sm_b])
            P.op("dve", lambda e: e.reciprocal(out=sm[:, 6:7], in_=sm[:, 5:6]), reads=[sm_b], writes=[sm_b])
            P.op("dve", lambda e: e.tensor_scalar(out=aff[:, i, :], in0=ex[:], scalar1=sm[:, 6:7], scalar2=None, op0=ALU.mult),
                 reads=[ex_b, sm_b], writes=[aff_b])
        for g in range(8):
            pt, pt_b = pR.next()
            for j in range(4):
                i = g * 4 + j
                P.op("pe", lambda e, j=j, i=i: e.transpose(out=pt[0:16, j * 128:(j + 1) * 128], in_=aff[:, i, :], identity=identf[:]),
                     reads=[aff_b, idf_b], writes=[pt_b], sig=(j == 3))
            P.op("act", lambda e: e.copy(out=affT[:, g * 512:(g + 1) * 512], in_=pt[0:16, :]), reads=[pt_b], writes=[affT_b])
        P.op("dve", lambda e: e.memset(bs[:], 0.0), writes=[bs_b])
        P.op("dve", lambda e: e.memset(bs[:, 1:2], 1.0), reads=[bs_b], writes=[bs_b])
        for it in range(30):
            P.op("dve", lambda e: e.tensor_scalar(out=bs[:, 2:3], in0=bs[:, 0:1], scalar1=bs[:, 1:2], scalar2=0.5, op0=ALU.add, op1=ALU.mult),
                 reads=[bs_b], writes=[bs_b])
            P.op("dve", lambda e: e.tensor_scalar(out=junk[:], in0=affT[:], scalar1=bs[:, 2:3], scalar2=0.0, op0=ALU.is_ge, op1=ALU.add,
                                                  accum_out=bs[:, 3:4]),
                 reads=[affT_b, bs_b], writes=[junk_b, bs_b])
            P.op("dve", lambda e: e.tensor_scalar(out=bs[:, 4:5], in0=bs[:, 3:4], scalar1=float(CAP), scalar2=None, op0=ALU.is_ge),
                 reads=[bs_b], writes=[bs_b])
            P.op("dve", lambda e: e.tensor_scalar(out=bs[:, 5:6], in0=bs[:, 4:5], scalar1=-1.0, scalar2=1.0, op0=ALU.mult, op1=ALU.add),
                 reads=[bs_b], writes=[bs_b])
            P.op("dve", lambda e: e.tensor_tensor(out=bs[:, 6:7], in0=bs[:, 0:1], in1=bs[:, 5:6], op=ALU.mult), reads=[bs_b], writes=[bs_b])
            P.op("dve", lambda e: e.scalar_tensor_tensor(out=bs[:, 0:1], in0=bs[:, 2:3], scalar=bs[:, 4:5], in1=bs[:, 6:7], op0=ALU.mult, op1=ALU.add),
                 reads=[bs_b], writes=[bs_b])
            P.op("dve", lambda e: e.tensor_tensor(out=bs[:, 6:7], in0=bs[:, 2:3], in1=bs[:, 5:6], op=ALU.mult), reads=[bs_b], writes=[bs_b])
            P.op("dve", lambda e: e.scalar_tensor_tensor(out=bs[:, 1:2], in0=bs[:, 1:2], scalar=bs[:, 4:5], in1=bs[:, 6:7], op0=ALU.mult, op1=ALU.add),
                 reads=[bs_b], writes=[bs_b])
        P.op("dve", lambda e: e.tensor_scalar(out=bs[:, 8:16], in0=identf[0:16, 0:8], scalar1=bs[:, 0:1], scalar2=None, op0=ALU.mult),
             reads=[bs_b, idf_b], writes=[bs_b])
        ptau, ptau_b = pR.next()
        P.op("pe", lambda e: e.matmul(ptau[:, 0:8], lhsT=ones_f[0:16, :], rhs=bs[:, 8:16], start=True, stop=True),
             reads=[ones_fb, bs_b], writes=[ptau_b])
        tauB, tauB_b = cx.sb("tauB", [128, 8], F32, phA)
        P.op("dve", lambda e: e.tensor_copy(out=tauB[:], in_=ptau[:, 0:8]), reads=[ptau_b], writes=[tauB_b])
        P.op("dve", lambda e: e.tensor_tensor(out=mask[:, :, 0:8], in0=aff[:, :, 0:8], in1=tauB[:].unsqueeze(1).broadcast_to([128, 32, 8]), op=ALU.is_ge),
             reads=[aff_b, tauB_b], writes=[mask_b])
        sa, sa_b = cx.sb("sa", [128, 32, 8], F32, phA)
        sb2, sb2_b = cx.sb("sb2", [128, 32, 8], F32, phA)
        P.op("dve", lambda e: e.tensor_copy(out=sa[:], in_=mask[:, :, 0:8]), reads=[mask_b], writes=[sa_b])
        cur, cur_b, nxt, nxt_b = sa, sa_b, sb2, sb2_b
        for sh in (1, 2, 4, 8, 16):
            P.op("dve", lambda e, sh=sh, cur=cur, nxt=nxt: e.tensor_copy(out=nxt[:, 0:sh, :], in_=cur[:, 0:sh, :]), reads=[cur_b], writes=[nxt_b])
            P.op("dve", lambda e, sh=sh, cur=cur, nxt=nxt: e.tensor_tensor(out=nxt[:, sh:32, :], in0=cur[:, sh:32, :], in1=cur[:, 0:32 - sh, :], op=ALU.add),
                 reads=[cur_b], writes=[nxt_b])
            cur, cur_b, nxt, nxt_b = nxt, nxt_b, cur, cur_b
        poff, poff_b = pR.next()
        P.op("pe", lambda e: e.matmul(poff[:, 0:8], lhsT=lts[:], rhs=cur[:, 31, :], start=True, stop=True), reads=[lts_b, cur_b], writes=[poff_b])
        offs, offs_b = cx.sb("offs", [128, 8], F32, phA)
        P.op("dve", lambda e: e.tensor_copy(out=offs[:], in_=poff[:, 0:8]), reads=[poff_b], writes=[offs_b])
        P.op("dve", lambda e: e.tensor_tensor(out=pos[:, :, 0:8], in0=cur[:], in1=mask[:, :, 0:8], op=ALU.subtract), reads=[cur_b, mask_b], writes=[pos_b])
        P.op("dve", lambda e: e.tensor_tensor(out=pos[:, :, 0:8], in0=pos[:, :, 0:8], in1=offs[:].unsqueeze(1).broadcast_to([128, 32, 8]), op=ALU.add),
             reads=[pos_b, offs_b], writes=[pos_b])
        P.barrier()
        phA.close()
        phC = ExitStack()
        wbufs = Rot([cx.sb("wb%d" % i, [128, 16, 512], BF16, phC) for i in range(4)])
        xes = [cx.sb("xe%d" % i, [128, D], BF16, phC) for i in range(4)]
        xeT, xeT_b = cx.sb("xeT", [128, 16, 512], BF16, phC)
        hidT, hidT_b = cx.sb("hidT", [128, 16, 512], BF16, phC)
        ysb = [cx.sb("ysb%d" % i, [128, D], F32, phC) for i in range(4)]
        OHs = Rot([cx.sb("oh%d" % i, [128, 512], F32, phC) for i in range(2)])
        tv, tv_b = cx.sb("tv", [128, 32, 2], F32, phC)
        ig, ig_b = cx.sb("ig", [2, 512], F32, phC)
        igT, igT_b = cx.sb("igT", [128, 4, 2], F32, phC)
        idxs = Rot([cx.sb("idx%d" % i, [128, 4], I32, phC) for i in range(2)])
        sgs = Rot([cx.sb("sg%d" % i, [128, 512], F32, phC) for i in range(2)])
        P.op("dve", lambda e: e.tensor_copy(out=tv[:, :, 0], in_=tokid[:]), reads=[tokid_b], writes=[tv_b])

        def load_piece(src2d):
            wb_, wb_b = wbufs.next()
            v = src2d.rearrange("(k p) n -> p k n", p=128)
            P.dma("pool", wb_[:, 0:8, :], v[:, 0:8, :], wb_b, writes=[wb_b])
            P.dma("pool", wb_[:, 8:16, :], v[:, 8:16, :], wb_b, writes=[wb_b])
            return wb_, wb_b

        for el in range(NEL):
            P.op("dve", lambda e: e.tensor_copy(out=tv[:, :, 1], in_=aff[:, :, el]), reads=[aff_b], writes=[tv_b])
            pig, pig_b = pR.next()
            for i in range(32):
                oh, oh_b = OHs.next()
                P.op("dve", lambda e, i=i: e.tensor_scalar(out=oh[:], in0=iota[:], scalar1=pos[:, i, el:el + 1], scalar2=mask[:, i, el:el + 1],
                                                          op0=ALU.is_equal, op1=ALU.mult),
                     reads=[iota_b, pos_b, mask_b], writes=[oh_b])
                P.op("pe", lambda e, i=i: e.matmul(pig[0:2, :], lhsT=tv[:, i, :], rhs=oh[:], start=(i == 0), stop=(i == 31)),
                     reads=[tv_b, oh_b], writes=[pig_b], sig=True)
            P.op("act", lambda e: e.copy(out=ig[:], in_=pig[0:2, :]), reads=[pig_b], writes=[ig_b])
            pgt, pgt_b = pR.next()
            for st in range(4):
                P.op("pe", lambda e, st=st: e.transpose(out=pgt[:, st * 2:st * 2 + 2], in_=ig[:, st * 128:(st + 1) * 128], identity=identf[0:2, 0:2]),
                     reads=[ig_b, idf_b], writes=[pgt_b], sig=(st == 3))
            P.op("dve", lambda e: e.tensor_copy(out=igT[:], in_=pgt[:, 0:8].rearrange("p (s t) -> p s t", t=2)), reads=[pgt_b], writes=[igT_b])
            idx, idx_b = idxs.next()
            P.op("dve", lambda e: e.tensor_copy(out=idx[:], in_=igT[:, :, 0]), reads=[igT_b], writes=[idx_b])
            gate = igT
            for st in range(4):
                xe, xe_b = xes[st]
                P.idma(xe[:], None, h2d, bass.IndirectOffsetOnAxis(ap=idx[:, st:st + 1], axis=0), xe_b, reads=[idx_b, h2d_b], writes=[xe_b])
                for g in range(2):
                    emit_tgroup(P, tps, identb[:], idb_b, [xe[:, (g * 8 + j) * 128:(g * 8 + j + 1) * 128] for j in range(8)], xe_b, 128,
                                xeT[:, g * 8:(g + 1) * 8, st * 128:(st + 1) * 128], xeT_b, eng=("act" if g == 0 else "dve"))
            for fb in range(4):
                G, G_b = load_piece(wg[el][:, fb * 512:(fb + 1) * 512])
                U, U_b = load_piece(wu[el][:, fb * 512:(fb + 1) * 512])
                for fc in range(4):
                    pg, pg_b = pR.next()
                    pu, pu_b = pR.next()
                    for (W, W_b, pp, pp_b) in ((G, G_b, pg, pg_b), (U, U_b, pu, pu_b)):
                        for k in range(16):
                            P.op("pe", lambda e, k=k, W=W, pp=pp: e.matmul(pp[:], lhsT=W[:, k, fc * 128:(fc + 1) * 128], rhs=xeT[:, k, :],
                                                                          start=(k == 0), stop=(k == 15)),
                                 reads=[W_b, xeT_b], writes=[pp_b], sig=(k == 15))
                    sg, sg_b = sgs.next()
                    P.op("act", lambda e: e.activation(out=sg[:], in_=pg[:], func=AF.Silu), reads=[pg_b], writes=[sg_b])
                    P.op("dve", lambda e: e.tensor_tensor(out=hidT[:, fb * 4 + fc, :], in0=sg[:], in1=pu[:], op=ALU.mult),
                         reads=[sg_b, pu_b], writes=[hidT_b])
            for db in range(4):
                Dw, Dw_b = load_piece(wd[el][:, db * 512:(db + 1) * 512])
                for st in range(4):
                    py, py_b = pR.next()
                    for f in range(16):
                        P.op("pe", lambda e, f=f: e.matmul(py[:], lhsT=hidT[:, f, st * 128:(st + 1) * 128], rhs=Dw[:, f, :],
                                                           start=(f == 0), stop=(f == 15)),
                             reads=[hidT_b, Dw_b], writes=[py_b], sig=(f == 15))
                    yt, yt_b = ysb[st]
                    P.op("act", lambda e: e.activation(out=yt[:, db * 512:(db + 1) * 512], in_=py[:], func=AF.Copy, scale=gate[:, st, 1:2]),
                         reads=[py_b, igT_b], writes=[yt_b])
            P._deps("pool", (), accS)
            for st in range(4):
                yt, yt_b = ysb[st]
                P.idma(acc, bass.IndirectOffsetOnAxis(ap=idx[:, st:st + 1], axis=0), yt[:], None, yt_b,
                       reads=[idx_b, yt_b], writes=[accS[st]], compute_op=ALU.add)
        P.finish()
        phC.close()

    ident_np = np.eye(128, dtype=np.float32)
    lts_np = np.triu(np.ones((128, 128), np.float32), 1)
    tokid_np = (np.arange(32)[None, :] * 128 + np.arange(128)[:, None]).astype(np.float32)
    iota_np = np.ascontiguousarray(np.broadcast_to(np.arange(512, dtype=np.float32)[None, :], (128, 512)))
    in_maps = []
    for c in range(NCORES):
        b, hf = c // 2, c % 2
        own = list(range(hf * NEL, (hf + 1) * NEL))
        oth = [e for e in range(16) if e not in own]
        in_maps.append({
            "xin": np.ascontiguousarray(xfull[b].reshape(32, 128, D)),
            "mv": np.ascontiguousarray(modv[c][layer]),
            "rw": np.ascontiguousarray(inputs["router_w"][layer][:, own + oth]),
            "wg": inputs["expert_w_gate"][layer, hf * NEL:(hf + 1) * NEL],
            "wu": inputs["expert_w_up"][layer, hf * NEL:(hf + 1) * NEL],
            "wd": inputs["expert_w_down"][layer, hf * NEL:(hf + 1) * NEL],
            "ident": ident_np, "lts": lts_np, "tokid": tokid_np, "iota": iota_np,
        })
    res = run_prog(cx, body, in_maps)
    return [r["acc"] for r in res]


def stage_odd_in(inputs, modv, x1, accs):
    cx = Ctx()
    nc = cx.nc
    xin = cx.dram("xin", [16, 128, D], F32, "ExternalInput")
    pa = cx.dram("pa", [16, 128, D], F32, "ExternalInput")
    pb = cx.dram("pb", [16, 128, D], F32, "ExternalInput")
    mv0 = cx.dram("mv0", [8, D], F32, "ExternalInput")
    mv1 = cx.dram("mv1", [8, D], F32, "ExternalInput")
    w_in = cx.dram("w_in", [D, D], F32, "ExternalInput")
    identd = cx.dram("ident", [128, 128], F32, "ExternalInput")
    xout = cx.dram("xout", [16, 128, D], F32, "ExternalOutput")
    zp = cx.dram("zp", [TOWN, 1024], BF16, "ExternalOutput")
    zfT = cx.dram("zfT", [8, 128, TOWN], BF16, "ExternalOutput")
    cx.start()
    P = cx.P
    w, w_b = cx.sb("w", [128, 16, D], BF16)
    ident, id_b = cx.sb("identb", [128, 128], BF16)
    G2, G2_b = cx.sb("G2", [128, D], F32)
    A1, A1_b = cx.sb("A1", [128, D], F32)
    B1, B1_b = cx.sb("B1", [128, D], F32)
    eps, eps_b = cx.sb("eps", [128, 1], F32)
    sm, sm_b = cx.sb("sm", [128, 8], F32)
    xts = Rot([cx.sb("xt%d" % i, [128, D], F32) for i in range(2)])
    pas = Rot([cx.sb("pa%d" % i, [128, D], F32) for i in range(2)])
    pbs = Rot([cx.sb("pb%d" % i, [128, D], F32) for i in range(2)])
    hb, hb_b = cx.sb("hb", [128, D], BF16)
    hT4s = Rot([cx.sb("hT4_%d" % i, [128, 16, 512], BF16) for i in range(2)])
    zps = Rot([cx.sb("zps%d" % i, [128, 1024], BF16) for i in range(2)])
    zfs = Rot([cx.sb("zfs%d" % i, [128, 8, 512], BF16) for i in range(2)])
    tps = Rot([cx.ps("tp%d" % i, [128, 1024], BF16) for i in range(2)])
    pjs = Rot([cx.ps("pj%d" % i, [128, 512], F32) for i in range(6)])

    def body():
        P.dma("pool", ident[:], identd, id_b, writes=[id_b])
        P.op("dve", lambda e: e.memset(eps[:], EPS), writes=[eps_b])
        wv = w_in.rearrange("(k p) n -> p k n", p=128)
        for c0 in range(0, D, 512):
            P.dma("pool", w[:, :, c0:c0 + 512], wv[:, :, c0:c0 + 512], w_b, writes=[w_b])
        P.dma("sp", G2[:], mv0[5:6, :].broadcast_to([128, D]), G2_b, writes=[G2_b])
        P.dma("sp", A1[:], mv1[0:1, :].broadcast_to([128, D]), A1_b, writes=[A1_b])
        P.dma("sp", B1[:], mv1[1:2, :].broadcast_to([128, D]), B1_b, writes=[B1_b])
        for tg in range(4):
            hT4, hT4_b = hT4s.next()
            for tl in range(4):
                i = tg * 4 + tl
                xt, xt_b = xts.next()
                at, at_b = pas.next()
                bt, bt_b = pbs.next()
                P.dma("sp", xt[:], xin[i], xt_b, writes=[xt_b])
                P.dma("act", at[:], pa[i], at_b, writes=[at_b])
                P.dma("act", bt[:], pb[i], bt_b, writes=[bt_b])
                P.op("pool", lambda e: e.tensor_tensor(out=at[:], in0=at[:], in1=bt[:], op=ALU.add), reads=[at_b, bt_b], writes=[at_b])
                P.op("dve", lambda e: e.tensor_tensor(out=at[:], in0=at[:], in1=G2[:], op=ALU.mult), reads=[at_b, G2_b], writes=[at_b])
                P.op("pool", lambda e: e.tensor_tensor(out=xt[:], in0=xt[:], in1=at[:], op=ALU.add), reads=[xt_b, at_b], writes=[xt_b])
                P.dma("sp", xout[i], xt[:], xt_b, reads=[xt_b])
                emit_rmsnorm_stats(P, xt[:], xt_b, D, hb[:], hb_b, sm[:, 0:1], sm_b, sm[:, 1:2], sm_b, sm[:, 2:3], sm_b, eps[:], eps_b)
                P.op("dve", lambda e: e.scalar_tensor_tensor(out=at[:], in0=xt[:], scalar=sm[:, 2:3], in1=A1[:], op0=ALU.mult, op1=ALU.mult),
                     reads=[xt_b, sm_b, A1_b], writes=[at_b])
                P.op("dve", lambda e: e.tensor_tensor(out=hb[:], in0=at[:], in1=B1[:], op=ALU.add), reads=[at_b, B1_b], writes=[hb_b])
                for g in range(2):
                    emit_tgroup(P, tps, ident[:], id_b, [hb[:, (g * 8 + j) * 128:(g * 8 + j + 1) * 128] for j in range(8)], hb_b, 128,
                                hT4[:, g * 8:(g + 1) * 8, tl * 128:(tl + 1) * 128], hT4_b, eng="act")
                zs, zs_b = zps.next()
                for nb in range(2):
                    pj, pj_b = pjs.next()
                    for k in range(16):
                        P.op("pe", lambda e, k=k: e.matmul(pj[:], lhsT=hT4[:, k, tl * 128:(tl + 1) * 128], rhs=w[:, k, nb * 512:(nb + 1) * 512],
                                                           start=(k == 0), stop=(k == 15)),
                             reads=[hT4_b, w_b], writes=[pj_b], sig=(k == 15))
                    P.op("act", lambda e: e.copy(out=zs[:, nb * 512:(nb + 1) * 512], in_=pj[:]), reads=[pj_b], writes=[zs_b])
                P.dma("sp", zp[i * 128:(i + 1) * 128, :], zs[:], zs_b, reads=[zs_b])
            zf, zf_b = zfs.next()
            for ch in range(8):
                pj, pj_b = pjs.next()
                for k in range(16):
                    P.op("pe", lambda e, k=k: e.matmul(pj[:], lhsT=w[:, k, 1024 + ch * 128:1024 + (ch + 1) * 128], rhs=hT4[:, k, :],
                                                       start=(k == 0), stop=(k == 15)),
                         reads=[w_b, hT4_b], writes=[pj_b], sig=(k == 15))
                if ch % 2 == 0:
                    P.op("act", lambda e: e.copy(out=zf[:, ch, :], in_=pj[:]), reads=[pj_b], writes=[zf_b])
                else:
                    P.op("dve", lambda e: e.tensor_copy(out=zf[:, ch, :], in_=pj[:]), reads=[pj_b], writes=[zf_b])
            P.dma("sp", zfT[:, :, tg * 512:(tg + 1) * 512].rearrange("c p t -> p c t"), zf[:], zf_b, reads=[zf_b])

    ident_np = np.eye(128, dtype=np.float32)
    in_maps = []
    for c in range(NCORES):
        b, hf = c // 2, c % 2
        rows = slice(hf * TOWN, (hf + 1) * TOWN)
        in_maps.append({
            "xin": np.ascontiguousarray(x1[c].reshape(16, 128, D)),
            "pa": np.ascontiguousarray(accs[2 * b][rows].reshape(16, 128, D)),
            "pb": np.ascontiguousarray(accs[2 * b + 1][rows].reshape(16, 128, D)),
            "mv0": np.ascontiguousarray(modv[c][0]), "mv1": np.ascontiguousarray(modv[c][1]),
            "w_in": inputs["odd_w_in"][0], "ident": ident_np,
        })
    return run_prog(cx, body, in_maps)


def band_np(gi):
    band = np.zeros((4, 3, 128, 128), np.float32)
    invc = np.zeros((4, 128), np.float32)
    for g, wdw in enumerate((2, 4, 8, 16)):
        half = wdw // 2
        for t in range(128):
            T = gi * 128 + t
            lo, hi = max(T - half, 0), min(T + half, SEQ)
            invc[g, t] = 1.0 / (hi - lo)
            for T2 in range(lo, hi):
                rel = T2 // 128 - gi
                band[g, rel + 1, T2 % 128, t] += 1.0
            band[g, 1, t, t] -= float(hi - lo)
    return band, invc


def stage_odd_mix(inputs, modv, s6):
    cx = Ctx()
    nc = cx.nc
    xin = cx.dram("xin", [16, 128, D], F32, "ExternalInput")
    zpe = cx.dram("zpe", [18, 128, 1024], BF16, "ExternalInput")
    zfTf = cx.dram("zfTf", [8, 128, SEQ], BF16, "ExternalInput")
    bandd = cx.dram("band", [3, 4, 3, 128, 128], F32, "ExternalInput")
    invcd = cx.dram("invc", [3, 4, 128], F32, "ExternalInput")
    csd = cx.dram("cs", [2, 128, 512], F32, "ExternalInput")
    cnT = cx.dram("cnT", [32, 128, TOWN], BF16, "ExternalInput")
    snT = cx.dram("snT", [32, 128, TOWN], BF16, "ExternalInput")
    pool_w = cx.dram("pool_w", [4, 256, 256], F32, "ExternalInput")
    pool_s = cx.dram("pool_s", [1024], F32, "ExternalInput")
    four_w = cx.dram("four_w", [4, 256, 256], F32, "ExternalInput")
    w_out = cx.dram("w_out", [D, D], F32, "ExternalInput")
    mv1 = cx.dram("mv1", [8, D], F32, "ExternalInput")
    xout = cx.dram("xout", [16, 128, D], F32, "ExternalOutput")
    cx.start()
    P = cx.P
    catT, cat_b = cx.sb("catT", [128, 16, TOWN], BF16)
    pbanks = [cx.ps("pb%d" % i, [128, 512], F32) for i in range(8)]
    pR = Rot(pbanks[0:6])
    pSp = pbanks[6:8]

    def body():
        ph1 = ExitStack()
        zt, zt_b = cx.sb("zt", [128, 18, 1024], BF16, ph1)
        band, band_b = cx.sb("band", [128, 3, 4, 3, 128], BF16, ph1)
        invc, invc_b = cx.sb("invc", [128, 3, 4, 128], F32, ph1)
        pooledT, pl_b = cx.sb("pooledT", [128, 8, TOWN], BF16, ph1)
        pw, pw_b = cx.sb("pw", [128, 4, 2, 256], BF16, ph1)
        pst, pst_b = cx.sb("pst", [128, 8], F32, ph1)
        for i in range(18):
            P.dma("sp", zt[:, i, :], zpe[i], zt_b, writes=[zt_b])
        P.dma("pool", band[:], bandd.rearrange("y g r p t -> p y g r t"), band_b, writes=[band_b])
        for y in range(3):
            for g in range(4):
                P.dma("act", invc[:, y, g, :], invcd[y, g:g + 1, :].broadcast_to([128, 128]), invc_b, writes=[invc_b])
        P.dma("pool", pw[:], pool_w.rearrange("g (c p) d -> p g c d", p=128), pw_b, writes=[pw_b])
        for ch in range(8):
            P.dma("sp", pst[:, ch:ch + 1], pool_s[ch * 128:(ch + 1) * 128].rearrange("(p o) -> p o", o=1), pst_b, writes=[pst_b])
        for i in range(16):
            y = 0 if i == 0 else (2 if i == 15 else 1)
            for g in range(4):
                pp, pp_b = pR.next()
                for cc in range(2):
                    for r in range(3):
                        P.op("pe", lambda e, cc=cc, r=r: e.matmul(pp[:, cc * 128:(cc + 1) * 128],
                                                                 lhsT=zt[:, i + r, g * 256 + cc * 128:g * 256 + (cc + 1) * 128],
                                                                 rhs=band[:, y, g, r, :], start=(r == 0), stop=(r == 2)),
                             reads=[zt_b, band_b], writes=[pp_b], sig=(cc == 1 and r == 2))
                P.op("dve", lambda e: e.tensor_tensor(out=pooledT[:, g * 2:g * 2 + 2, i * 128:(i + 1) * 128],
                                                      in0=pp[:, 0:256].rearrange("p (c t) -> p c t", c=2),
                                                      in1=invc[:, y, g, :].unsqueeze(1).broadcast_to([128, 2, 128]), op=ALU.mult),
                     reads=[pp_b, invc_b], writes=[pl_b])
        for g in range(4):
            for dc in range(2):
                for tb in range(4):
                    pj, pj_b = pR.next()
                    for cc in range(2):
                        P.op("pe", lambda e, cc=cc: e.matmul(pj[:], lhsT=pw[:, g, cc, dc * 128:(dc + 1) * 128],
                                                             rhs=pooledT[:, g * 2 + cc, tb * 512:(tb + 1) * 512], start=(cc == 0), stop=(cc == 1)),
                             reads=[pw_b, pl_b], writes=[pj_b], sig=(cc == 1))
                    P.op("act", lambda e: e.activation(out=catT[:, g * 2 + dc, tb * 512:(tb + 1) * 512], in_=pj[:], func=AF.Copy,
                                                       scale=pst[:, g * 2 + dc:g * 2 + dc + 1]),
                         reads=[pj_b, pst_b], writes=[cat_b])
        P.barrier()
        ph1.close()
        ph2 = ExitStack()
        UV, UV_b = cx.sb("UV", [128, 32, 512], BF16, ph2)
        zfs = Rot([cx.sb("zf%d" % i, [128, 2, SEQ], BF16, ph2) for i in range(1)])
        tabs = Rot([cx.sb("tab%d" % i, [128, 4, 2, 512], BF16, ph2) for i in range(4)])
        cs, cs_b = cx.sb("cs", [128, 2, 512], BF16, ph2)
        fw, fw_b = cx.sb("fw", [128, 4, 2, 256], BF16, ph2)
        spT, spT_b = cx.sb("spT", [128, 2, 512], BF16, ph2)
        P.dma("pool", cs[:], csd.rearrange("c p n -> p c n"), cs_b, writes=[cs_b])
        P.dma("pool", fw[:], four_w.rearrange("g (c p) d -> p g c d", p=128), fw_b, writes=[fw_b])
        for g in range(4):
            zf, zf_b = zfs.next()
            for cc in range(2):
                P.dma("sp", zf[:, cc, :], zfTf[g * 2 + cc], zf_b, writes=[zf_b])
            for ti in range(32):
                pj, pj_b = pR.next()
                for cc in range(2):
                    P.op("pe", lambda e, cc=cc: e.matmul(pj[:], lhsT=zf[:, cc, ti * 128:(ti + 1) * 128], rhs=cs[:, cc, :],
                                                         start=(cc == 0), stop=(cc == 1)),
                         reads=[zf_b, cs_b], writes=[pj_b], sig=(cc == 1))
                if ti % 2 == 0:
                    P.op("act", lambda e: e.copy(out=UV[:, ti, :], in_=pj[:]), reads=[pj_b], writes=[UV_b])
                else:
                    P.op("dve", lambda e: e.tensor_copy(out=UV[:, ti, :], in_=pj[:]), reads=[pj_b], writes=[UV_b])
            for kb in range(4):
                for c8 in range(8):
                    tb_, tb_b = tabs.next()
                    P.dma("sp", tb_[:, :, 0, :], cnT[c8 * 4:(c8 + 1) * 4, :, kb * 512:(kb + 1) * 512].rearrange("i p k -> p i k"), tb_b, writes=[tb_b])
                    P.dma("act", tb_[:, :, 1, :], snT[c8 * 4:(c8 + 1) * 4, :, kb * 512:(kb + 1) * 512].rearrange("i p k -> p i k"), tb_b, writes=[tb_b])
                    for tl in range(4):
                        ti = c8 * 4 + tl
                        for mc in range(2):
                            for s_ in range(2):
                                first = (ti == 0 and s_ == 0)
                                last = (ti == 31 and s_ == 1)
                                P.op("pe", lambda e, mc=mc, s_=s_: e.matmul(pSp[mc][0][:], lhsT=UV[:, ti, s_ * 256 + mc * 128:s_ * 256 + (mc + 1) * 128],
                                                                         rhs=tb_[:, tl, s_, :], start=first, stop=last),
                                     reads=[UV_b, tb_b], writes=[pSp[mc][1]], sig=(s_ == 1 and mc == 1))
                for mc in range(2):
                    if mc == 0:
                        P.op("act", lambda e: e.copy(out=spT[:, mc, :], in_=pSp[mc][0][:]), reads=[pSp[mc][1]], writes=[spT_b])
                    else:
                        P.op("dve", lambda e: e.tensor_copy(out=spT[:, mc, :], in_=pSp[mc][0][:]), reads=[pSp[mc][1]], writes=[spT_b])
                for dc in range(2):
                    pj, pj_b = pR.next()
                    for mc in range(2):
                        P.op("pe", lambda e, mc=mc: e.matmul(pj[:], lhsT=fw[:, g, mc, dc * 128:(dc + 1) * 128], rhs=spT[:, mc, :],
                                                             start=(mc == 0), stop=(mc == 1)),
                             reads=[fw_b, spT_b], writes=[pj_b], sig=(mc == 1))
                    P.op("act", lambda e: e.copy(out=catT[:, 8 + g * 2 + dc, kb * 512:(kb + 1) * 512], in_=pj[:]), reads=[pj_b], writes=[cat_b])
        P.barrier()
        ph2.close()
        ph3 = ExitStack()
        wo, wo_b = cx.sb("wo", [128, 16, D], BF16, ph3)
        G1, G1_b = cx.sb("G1", [128, D], F32, ph3)
        xts = Rot([cx.sb("xt%d" % i, [128, D], F32, ph3) for i in range(2)])
        yts = Rot([cx.sb("yt%d" % i, [128, D], F32, ph3) for i in range(2)])
        wv = w_out.rearrange("(k p) n -> p k n", p=128)
        for c0 in range(0, D, 512):
            P.dma("pool", wo[:, :, c0:c0 + 512], wv[:, :, c0:c0 + 512], wo_b, writes=[wo_b])
        P.dma("sp", G1[:], mv1[2:3, :].broadcast_to([128, D]), G1_b, writes=[G1_b])
        for tt in range(16):
            xt, xt_b = xts.next()
            yt, yt_b = yts.next()
            P.dma("sp", xt[:], xin[tt], xt_b, writes=[xt_b])
            for nb in range(4):
                pj, pj_b = pR.next()
                for f in range(16):
                    P.op("pe", lambda e, f=f: e.matmul(pj[:], lhsT=catT[:, f, tt * 128:(tt + 1) * 128], rhs=wo[:, f, nb * 512:(nb + 1) * 512],
                                                       start=(f == 0), stop=(f == 15)),
                         reads=[cat_b, wo_b], writes=[pj_b], sig=(f == 15))
                cs_ = slice(nb * 512, (nb + 1) * 512)
                P.op("dve", lambda e: e.tensor_tensor(out=yt[:, cs_], in0=pj[:], in1=G1[:, cs_], op=ALU.mult), reads=[pj_b, G1_b], writes=[yt_b])
                P.op("pool", lambda e: e.tensor_tensor(out=yt[:, cs_], in0=yt[:, cs_], in1=xt[:, cs_], op=ALU.add), reads=[yt_b, xt_b], writes=[yt_b])
            P.dma("sp", xout[tt], yt[:], yt_b, reads=[yt_b])
        P.finish()
        ph3.close()

    m = np.arange(256)
    ang = (2.0 * np.pi / 256.0) * ((m[:, None] * m[None, :]) % 256)
    cc_ = (np.cos(ang) / 16.0).astype(np.float32)
    sc_ = (np.sin(ang) / 16.0).astype(np.float32)
    cs_np = np.ascontiguousarray(np.concatenate([cc_, sc_], axis=1).reshape(2, 128, 512))
    tt_ = np.arange(SEQ, dtype=np.int64)
    in_maps = []
    for c in range(NCORES):
        b, hf = c // 2, c % 2
        c0, c1 = 2 * b, 2 * b + 1
        zp_full = np.concatenate([s6[c0]["zp"], s6[c1]["zp"]], axis=0).reshape(32, 128, 1024)
        zero = np.zeros((1, 128, 1024), zp_full.dtype)
        ext = np.concatenate([zero, zp_full, zero], axis=0)[hf * 16:hf * 16 + 18]
        zf_full = np.concatenate([s6[c0]["zfT"], s6[c1]["zfT"]], axis=2)
        bands, invcs = [], []
        for gi in (hf * 16, hf * 16 + 1, hf * 16 + 15):
            bd, iv = band_np(gi)
            bands.append(bd)
            invcs.append(iv)
        kk = np.arange(hf * TOWN, (hf + 1) * TOWN, dtype=np.int64)
        a2 = (2.0 * np.pi / SEQ) * ((tt_[:, None] * kk[None, :]) % SEQ)
        cn_np = (np.cos(a2) / 64.0).astype(NPBF).reshape(32, 128, TOWN)
        sn_np = (-np.sin(a2) / 64.0).astype(NPBF).reshape(32, 128, TOWN)
        in_maps.append({
            "xin": np.ascontiguousarray(s6[c]["xout"]),
            "zpe": np.ascontiguousarray(ext), "zfTf": np.ascontiguousarray(zf_full),
            "band": np.ascontiguousarray(np.stack(bands)), "invc": np.ascontiguousarray(np.stack(invcs)),
            "cs": cs_np, "cnT": cn_np, "snT": sn_np,
            "pool_w": inputs["pool_w"][0], "pool_s": inputs["pool_scale"][0], "four_w": inputs["fourier_w"][0],
            "w_out": inputs["odd_w_out"][0], "mv1": np.ascontiguousarray(modv[c][1]),
        })
    res = run_prog(cx, body, in_maps)
    return [r["xout"].reshape(TOWN, D) for r in res]


def stage_final(inputs, modv, x3, accs):
    cx = Ctx()
    nc = cx.nc
    xin = cx.dram("xin", [16, 128, D], F32, "ExternalInput")
    pa = cx.dram("pa", [16, 128, D], F32, "ExternalInput")
    pb = cx.dram("pb", [16, 128, D], F32, "ExternalInput")
    mv1 = cx.dram("mv1", [8, D], F32, "ExternalInput")
    fn = cx.dram("fn", [1, D], F32, "ExternalInput")
    xout = cx.dram("xout", [16, 128, D], F32, "ExternalOutput")
    cx.start()
    P = cx.P
    G2, G2_b = cx.sb("G2", [128, D], F32)
    FN, FN_b = cx.sb("FN", [128, D], F32)
    eps, eps_b = cx.sb("eps", [128, 1], F32)
    sm, sm_b = cx.sb("sm", [128, 8], F32)
    junk, junk_b = cx.sb("junk", [128, D], BF16)
    xts = Rot([cx.sb("xt%d" % i, [128, D], F32) for i in range(2)])
    pas = Rot([cx.sb("pa%d" % i, [128, D], F32) for i in range(2)])
    pbs = Rot([cx.sb("pb%d" % i, [128, D], F32) for i in range(2)])

    def body():
        P.op("dve", lambda e: e.memset(eps[:], EPS), writes=[eps_b])
        P.dma("sp", G2[:], mv1[5:6, :].broadcast_to([128, D]), G2_b, writes=[G2_b])
        P.dma("sp", FN[:], fn.broadcast_to([128, D]), FN_b, writes=[FN_b])
        for i in range(16):
            xt, xt_b = xts.next()
            at, at_b = pas.next()
            bt, bt_b = pbs.next()
            P.dma("sp", xt[:], xin[i], xt_b, writes=[xt_b])
            P.dma("act", at[:], pa[i], at_b, writes=[at_b])
            P.dma("act", bt[:], pb[i], bt_b, writes=[bt_b])
            P.op("pool", lambda e: e.tensor_tensor(out=at[:], in0=at[:], in1=bt[:], op=ALU.add), reads=[at_b, bt_b], writes=[at_b])
            P.op("dve", lambda e: e.tensor_tensor(out=at[:], in0=at[:], in1=G2[:], op=ALU.mult), reads=[at_b, G2_b], writes=[at_b])
            P.op("pool", lambda e: e.tensor_tensor(out=xt[:], in0=xt[:], in1=at[:], op=ALU.add), reads=[xt_b, at_b], writes=[xt_b])
            emit_rmsnorm_stats(P, xt[:], xt_b, D, junk[:], junk_b, sm[:, 0:1], sm_b, sm[:, 1:2], sm_b, sm[:, 2:3], sm_b, eps[:], eps_b)
            P.op("dve", lambda e: e.scalar_tensor_tensor(out=bt[:], in0=xt[:], scalar=sm[:, 2:3], in1=FN[:], op0=ALU.mult, op1=ALU.mult),
                 reads=[xt_b, sm_b, FN_b], writes=[bt_b])
            P.dma("sp", xout[i], bt[:], bt_b, reads=[bt_b])

    in_maps = []
    for c in range(NCORES):
        b, hf = c // 2, c % 2
        rows = slice(hf * TOWN, (hf + 1) * TOWN)
        in_maps.append({
            "xin": np.ascontiguousarray(x3[c].reshape(16, 128, D)),
            "pa": np.ascontiguousarray(accs[2 * b][rows].reshape(16, 128, D)),
            "pb": np.ascontiguousarray(accs[2 * b + 1][rows].reshape(16, 128, D)),
            "mv1": np.ascontiguousarray(modv[c][1]), "fn": inputs["final_norm"][None],
        })
    res = run_prog(cx, body, in_maps)
    return [r["xout"].reshape(TOWN, D) for r in res]


def kernel(**inputs):
    inputs = {k: np.asarray(v) for k, v in inputs.items()}
    modv = stage_mod(inputs)
    s2 = stage_proj0(inputs, modv)
    x1 = stage_attn0(inputs, modv, s2)
    del s2
    xfull = [np.concatenate([x1[2 * b], x1[2 * b + 1]], axis=0) for b in range(NB)]
    acc0 = stage_ffn(inputs, modv, xfull, 0)
    s6 = stage_odd_in(inputs, modv, x1, acc0)
    del acc0, x1, xfull
    x3 = stage_odd_mix(inputs, modv, s6)
    del s6
    xfull = [np.concatenate([x3[2 * b], x3[2 * b + 1]], axis=0) for b in range(NB)]
    acc1 = stage_ffn(inputs, modv, xfull, 1)
    out = stage_final(inputs, modv, x3, acc1)
    return np.ascontiguousarray(np.stack(out).reshape(NB, SEQ, D).astype(np.float32))
```
